# Optimizing a Trainium2 kernel written in Bass

```python
import math
import jax, jax.numpy as jnp
from jax import lax
import numpy as np

D_MODEL = 2048
BATCH = 4
SEQ = 2048
DEPTH = 1

MLA_HEADS = 8
MLA_NOPE = 128
MLA_ROPE = 64
MLA_QK = MLA_NOPE + MLA_ROPE
MLA_V = 128
Q_LORA = 512
KV_LORA = 256
ROPE_THETA = 10000.0
Q_BLOCK = 128
ML_HEADS = 8
ML_DQK = 128
ML_DV = 128
ML_CONV = 4
ML_CHUNK = 64
F_BIAS_LO = 3.0
F_BIAS_HI = 6.0
MLA_W = MLA_HEADS * MLA_V
ML_W = ML_HEADS * ML_DV
N_GROUPS = 4
EXP_PER_GROUP = 8
N_EXPERTS = N_GROUPS * EXP_PER_GROUP
TOP_K = 2
D_FF_EXPERT = 512
EPS = 1e-6
IN_SIZES = (Q_LORA, KV_LORA, MLA_ROPE, 2 * ML_HEADS * ML_DQK, ML_HEADS * ML_DV, ML_W, ML_HEADS, ML_HEADS, D_MODEL, D_MODEL)
IN_COLS = Q_LORA + KV_LORA + MLA_ROPE + 2 * ML_HEADS * ML_DQK + ML_HEADS * ML_DV + ML_W + 2 * ML_HEADS + 2 * D_MODEL

kernel_name = "hybrid_mla_mlstm_hmoe_adaln"


def rmsnorm(x, g):
    xf = x.astype(jnp.float32)
    xf = xf * lax.rsqrt(jnp.mean(xf * xf, axis=-1, keepdims=True) + EPS)
    return (xf * g.astype(jnp.float32)).astype(x.dtype)


def rope_tables(positions):
    inv = ROPE_THETA ** (-jnp.arange(0, MLA_ROPE, 2, dtype=jnp.float32) / MLA_ROPE)
    ang = positions.astype(jnp.float32)[..., None] * inv
    return jnp.cos(ang), jnp.sin(ang)


def apply_rope(t, cos, sin):
    cos = cos[:, :, None, :].astype(t.dtype)
    sin = sin[:, :, None, :].astype(t.dtype)
    t1, t2 = jnp.split(t, 2, axis=-1)
    return jnp.concatenate([t1 * cos - t2 * sin, t1 * sin + t2 * cos], axis=-1)


def causal_conv(u, w, b):
    C = u.shape[-1]
    out = lax.conv_general_dilated(u, w[:, None, :].astype(u.dtype), window_strides=(1,), padding=((ML_CONV - 1, 0),), dimension_numbers=('NWC', 'WIO', 'NWC'), feature_group_count=C)
    return out + b.astype(u.dtype)


def mla_attention(cq, ckv, kpe, cos, sin, q_a_norm_g, w_uq, kv_a_norm_g, w_ukv, q_norm_g, k_norm_g):
    B, S, _ = cq.shape
    q = (rmsnorm(cq, q_a_norm_g) @ w_uq).reshape(B, S, MLA_HEADS, MLA_QK)
    kv = (rmsnorm(ckv, kv_a_norm_g) @ w_ukv).reshape(B, S, MLA_HEADS, MLA_NOPE + MLA_V)
    k_nope, v = kv[..., :MLA_NOPE], kv[..., MLA_NOPE:]
    k = jnp.concatenate([k_nope, jnp.broadcast_to(kpe[:, :, None, :], (B, S, MLA_HEADS, MLA_ROPE))], axis=-1)
    q = rmsnorm(q, q_norm_g)
    k = rmsnorm(k, k_norm_g)
    q = jnp.concatenate([q[..., :MLA_NOPE], apply_rope(q[..., MLA_NOPE:], cos, sin)], axis=-1)
    k = jnp.concatenate([k[..., :MLA_NOPE], apply_rope(k[..., MLA_NOPE:], cos, sin)], axis=-1)
    nb = S // Q_BLOCK
    q_blocks = q.reshape(B, nb, Q_BLOCK, MLA_HEADS, MLA_QK).transpose(1, 0, 2, 3, 4)
    key_pos = jnp.arange(S)
    scale = MLA_QK ** -0.5

    def attend(args):
        q_blk, blk = args
        s = jnp.einsum('bqhd,bkhd->bhqk', q_blk, k).astype(jnp.float32) * scale
        q_pos = blk * Q_BLOCK + jnp.arange(Q_BLOCK)
        s = jnp.where(key_pos[None, :] <= q_pos[:, None], s, -jnp.inf)
        p = jax.nn.softmax(s, axis=-1).astype(v.dtype)
        return jnp.einsum('bhqk,bkhd->bqhd', p, v)

    o = lax.map(attend, (q_blocks, jnp.arange(nb)))
    return o.transpose(1, 0, 2, 3, 4).reshape(B, S, MLA_W)


def mlstm_chunkwise(q, k, v, i_pre, f_pre):
    B, S, H, _ = q.shape
    nc = S // ML_CHUNK
    f32 = jnp.float32

    def chunks(t):
        return t.astype(f32).reshape(B, nc, ML_CHUNK, H, -1).transpose(1, 0, 3, 2, 4)

    def gate_chunks(t):
        return t.astype(f32).reshape(B, nc, ML_CHUNK, H).transpose(1, 0, 3, 2)

    qc = chunks(q)
    kc = chunks(k) * (ML_DQK ** -0.5)
    vc = chunks(v)
    log_i = gate_chunks(i_pre)
    b_cum = jnp.cumsum(gate_chunks(jax.nn.log_sigmoid(f_pre.astype(f32))), axis=-1)
    causal = jnp.tril(jnp.ones((ML_CHUNK, ML_CHUNK), dtype=bool))

    def step(carry, inp):
        C, n, m = carry
        q_, k_, v_, b_, li = inp
        log_w = jnp.where(causal, b_[..., :, None] - b_[..., None, :] + li[..., None, :], -jnp.inf)
        log_inter = b_ + m[..., None]
        m_t = jnp.maximum(jnp.max(log_w, axis=-1), log_inter)
        w = jnp.exp(log_w - m_t[..., None])
        a = jnp.exp(log_inter - m_t)
        s = jnp.einsum('bhtd,bhsd->bhts', q_, k_) * w
        num = jnp.einsum('bhts,bhse->bhte', s, v_) + a[..., None] * jnp.einsum('bhtd,bhde->bhte', q_, C)
        den = jnp.sum(s, axis=-1) + a * jnp.einsum('bhtd,bhd->bht', q_, n)
        h = num / jnp.maximum(jnp.abs(den), jnp.exp(-m_t))[..., None]
        b_last = b_[..., -1]
        log_g = b_last[..., None] - b_ + li
        m_new = jnp.maximum(b_last + m, jnp.max(log_g, axis=-1))
        g = jnp.exp(log_g - m_new[..., None])
        decay = jnp.exp(b_last + m - m_new)
        C = decay[..., None, None] * C + jnp.einsum('bhs,bhsd,bhse->bhde', g, k_, v_)
        n = decay[..., None] * n + jnp.einsum('bhs,bhsd->bhd', g, k_)
        return (C, n, m_new), h

    init = (jnp.zeros((B, H, ML_DQK, ML_DV), f32), jnp.zeros((B, H, ML_DQK), f32), jnp.zeros((B, H), f32))
    _, h = lax.scan(step, init, (qc, kc, vc, b_cum, log_i))
    return h.transpose(1, 0, 3, 2, 4).reshape(B, S, H, ML_DV).astype(v.dtype)


def hybrid_mixer(h, cos, sin, w_in, q_a_norm_g, w_uq, kv_a_norm_g, w_ukv, q_norm_g, k_norm_g, conv_w, conv_b, b_mlstm_gates, mlstm_norm_g, w_proj_a, w_proj_b, w_out):
    B, S, _ = h.shape
    proj = h @ w_in
    split_at = np.cumsum(IN_SIZES)[:-1].tolist()
    cq, ckv, kpe, ml_qk, ml_v, ml_o, ml_i, ml_f, g_a, g_b = jnp.split(proj, split_at, axis=-1)
    out_a = mla_attention(cq, ckv, kpe, cos, sin, q_a_norm_g, w_uq, kv_a_norm_g, w_ukv, q_norm_g, k_norm_g)
    qk = jax.nn.silu(causal_conv(ml_qk, conv_w, conv_b))
    ml_q, ml_k = jnp.split(qk, 2, axis=-1)
    ml_q = ml_q.reshape(B, S, ML_HEADS, ML_DQK)
    ml_k = ml_k.reshape(B, S, ML_HEADS, ML_DQK)
    ml_v = ml_v.reshape(B, S, ML_HEADS, ML_DV)
    i_pre = ml_i + b_mlstm_gates[0].astype(ml_i.dtype)
    f_pre = ml_f + b_mlstm_gates[1].astype(ml_f.dtype)
    hm = mlstm_chunkwise(ml_q, ml_k, ml_v, i_pre, f_pre)
    hm = rmsnorm(hm, mlstm_norm_g.reshape(ML_HEADS, ML_DV)).reshape(B, S, ML_W) * jax.nn.sigmoid(ml_o)
    mixed = jax.nn.sigmoid(g_a) * (out_a @ w_proj_a) + jax.nn.sigmoid(g_b) * (hm @ w_proj_b)
    return mixed @ w_out


def hier_moe(h, w_group, b_group, w_router, b_router, w_gate_e, w_up_e, w_down_e):
    B, S, D = h.shape
    t = h.reshape(B * S, D)
    T = t.shape[0]
    g_logits = (t @ w_group).astype(jnp.float32) + b_group.astype(jnp.float32)
    g_prob = jax.nn.softmax(g_logits, axis=-1)
    g_sel = jnp.argmax(g_logits, axis=-1)
    g_w = jnp.take_along_axis(g_prob, g_sel[:, None], axis=-1)[:, 0]
    e_logits = ((t @ w_router).astype(jnp.float32) + b_router.astype(jnp.float32)).reshape(T, N_GROUPS, EXP_PER_GROUP)
    e_in = jnp.take_along_axis(e_logits, g_sel[:, None, None], axis=1)[:, 0]
    top_p, top_i = lax.top_k(jax.nn.softmax(e_in, axis=-1), TOP_K)
    weights = g_w[:, None] * top_p / jnp.sum(top_p, axis=-1, keepdims=True)
    expert_idx = g_sel[:, None] * EXP_PER_GROUP + top_i
    comb = jnp.sum(jax.nn.one_hot(expert_idx, N_EXPERTS, dtype=jnp.float32) * weights[..., None], axis=1).astype(t.dtype)
    out = jnp.zeros_like(t)
    for gi in range(N_GROUPS):
        sl = slice(gi * EXP_PER_GROUP, (gi + 1) * EXP_PER_GROUP)
        hg = jnp.einsum('td,edf->tef', t, w_gate_e[sl])
        hu = jnp.einsum('td,edf->tef', t, w_up_e[sl])
        act = jax.nn.silu(hg) * hu * comb[:, sl, None]
        out = out + jnp.einsum('tef,efd->td', act, w_down_e[sl])
    return out.reshape(B, S, D)


def setup_inputs(seed: int = 0) -> dict:
    key = jax.random.key(seed)
    ks = jax.random.split(key, 32)
    L = DEPTH
    f32 = jnp.float32

    def nrm(k, shape, fan_in):
        return jax.random.normal(k, shape, f32) * (fan_in ** -0.5)

    def gain(k, shape):
        return 1.0 + 0.02 * jax.random.normal(k, shape, f32)

    def bias(k, shape, s=0.01):
        return s * jax.random.normal(k, shape, f32)

    f_bias = jnp.linspace(F_BIAS_LO, F_BIAS_HI, ML_HEADS, dtype=f32)
    b_mlstm_gates = jnp.stack([bias(ks[13], (L, ML_HEADS), 0.1), f_bias[None, :] + bias(ks[14], (L, ML_HEADS), 0.1)], axis=1)
    positions = jnp.arange(SEQ, dtype=jnp.int32)[None, :] + jax.random.randint(ks[2], (BATCH, 1), 0, 1024, dtype=jnp.int32)
    return {
        "x": jax.random.normal(ks[0], (BATCH, SEQ, D_MODEL), f32),
        "c": jax.random.normal(ks[1], (BATCH, D_MODEL), f32),
        "positions": positions,
        "w_ada": nrm(ks[3], (L, D_MODEL, 6 * D_MODEL), D_MODEL),
        "b_ada": bias(ks[4], (L, 6 * D_MODEL), 0.02),
        "norm_mix_g": gain(ks[5], (L, D_MODEL)),
        "w_in": nrm(ks[6], (L, D_MODEL, IN_COLS), D_MODEL),
        "q_a_norm_g": gain(ks[7], (L, Q_LORA)),
        "w_uq": nrm(ks[8], (L, Q_LORA, MLA_HEADS * MLA_QK), Q_LORA),
        "kv_a_norm_g": gain(ks[9], (L, KV_LORA)),
        "w_ukv": nrm(ks[10], (L, KV_LORA, MLA_HEADS * (MLA_NOPE + MLA_V)), KV_LORA),
        "q_norm_g": gain(ks[11], (L, MLA_QK)),
        "k_norm_g": gain(ks[12], (L, MLA_QK)),
        "conv_w": nrm(ks[15], (L, ML_CONV, 2 * ML_HEADS * ML_DQK), ML_CONV),
        "conv_b": bias(ks[16], (L, 2 * ML_HEADS * ML_DQK)),
        "b_mlstm_gates": b_mlstm_gates,
        "mlstm_norm_g": gain(ks[17], (L, ML_W)),
        "w_proj_a": nrm(ks[18], (L, MLA_W, D_MODEL), MLA_W),
        "w_proj_b": nrm(ks[19], (L, ML_W, D_MODEL), ML_W),
        "w_out": nrm(ks[20], (L, D_MODEL, D_MODEL), D_MODEL),
        "norm_ffn_g": gain(ks[21], (L, D_MODEL)),
        "w_group": nrm(ks[22], (L, D_MODEL, N_GROUPS), D_MODEL),
        "b_group": bias(ks[23], (L, N_GROUPS)),
        "w_router": nrm(ks[24], (L, D_MODEL, N_EXPERTS), D_MODEL),
        "b_router": bias(ks[25], (L, N_EXPERTS)),
        "w_gate_e": nrm(ks[26], (L, N_EXPERTS, D_MODEL, D_FF_EXPERT), D_MODEL),
        "w_up_e": nrm(ks[27], (L, N_EXPERTS, D_MODEL, D_FF_EXPERT), D_MODEL),
        "w_down_e": nrm(ks[28], (L, N_EXPERTS, D_FF_EXPERT, D_MODEL), D_FF_EXPERT),
    }


def reference(x, c, positions, w_ada, b_ada, norm_mix_g, w_in, q_a_norm_g, w_uq, kv_a_norm_g, w_ukv, q_norm_g, k_norm_g, conv_w, conv_b, b_mlstm_gates, mlstm_norm_g, w_proj_a, w_proj_b, w_out, norm_ffn_g, w_group, b_group, w_router, b_router, w_gate_e, w_up_e, w_down_e):
    cos, sin = rope_tables(positions)
    cond = jax.nn.silu(c)
    for l in range(DEPTH):
        mod = cond @ w_ada[l] + b_ada[l]
        sh_a, sc_a, gt_a, sh_m, sc_m, gt_m = [m[:, None, :] for m in jnp.split(mod, 6, axis=-1)]
        h = rmsnorm(x, norm_mix_g[l]) * (1.0 + sc_a) + sh_a
        x = x + gt_a * hybrid_mixer(h, cos, sin, w_in[l], q_a_norm_g[l], w_uq[l], kv_a_norm_g[l], w_ukv[l], q_norm_g[l], k_norm_g[l], conv_w[l], conv_b[l], b_mlstm_gates[l], mlstm_norm_g[l], w_proj_a[l], w_proj_b[l], w_out[l])
        h = rmsnorm(x, norm_ffn_g[l]) * (1.0 + sc_m) + sh_m
        x = x + gt_m * hier_moe(h, w_group[l], b_group[l], w_router[l], b_router[l], w_gate_e[l], w_up_e[l], w_down_e[l])
    return x
```

```python
import math
import numpy as np
import concourse.bass as bass
import concourse.mybir as mybir
from concourse.bass_utils import run_bass_kernel_spmd
from concourse.alu_op_type import AluOpType as ALU

dt = mybir.dt
AF = mybir.ActivationFunctionType
F32 = dt.float32
BF16 = dt.bfloat16
I32 = dt.int32

D = 2048
S = 2048
NB = 4
T = 1024
KC = 16
H = 8
EPS = 1e-6
N_EXP = 32
DFF = 512
C_HEAD, C_G, C_CQ, C_CKV, C_KPE, C_KPES, C_I = 0, 4096, 8192, 8704, 8960, 9024, 9088
NCOL2 = 9104
LN_KSCALE = math.log(128 ** -0.5)
NEG = -30000.0

_DSIZE = {F32: 4, BF16: 2, I32: 4}


def _dsz(d):
    return _DSIZE[d]


class _Op:
    __slots__ = ("eng", "fn", "idx", "gidx", "dma", "deps", "tok", "need_tok", "dma_slot", "dma_val", "waits")

    def __init__(self, eng, fn, dma):
        self.eng = eng
        self.fn = fn
        self.dma = dma
        self.deps = []
        self.tok = None
        self.need_tok = False
        self.dma_slot = None
        self.dma_val = 0
        self.waits = []


def _region(ap):
    t = ap.tensor
    name = t.name
    shape = list(t.shape)
    dsz = _dsz(ap.dtype)
    row = 1
    for s in shape[1:]:
        row *= int(s)
    off = int(ap.offset)
    apl = [(int(a), int(b)) for a, b in ap.ap]
    pstep, pcnt = apl[0]
    p0 = off // row
    f0 = off % row
    ext = 1
    for s, c in apl[1:]:
        ext += (c - 1) * abs(s)
    p1 = p0 + (pcnt if pstep != 0 else 1)
    if name == "ps":
        bk = (f0 * dsz) // 2048
        return (name, 0, 128, bk * 2048, (bk + 1) * 2048)
    return (name, p0, p1, f0 * dsz, (f0 + ext) * dsz)


class Prog:
    ENG = ("pe", "act", "dve", "pool", "sp")
    ROT = 1500
    NSLOT = 8
    BIN = 2048

    def nslot(self, e):
        return 2 if e == "pool" else self.NSLOT

    def __init__(self, nc):
        self.nc = nc
        self.ops = []
        self.per = {e: [] for e in self.ENG}
        self.bins = {}
        self.dram_last = {}
        self.final_dma = []

    @staticmethod
    def _ovl(a, b):
        return a[1] < b[2] and b[1] < a[2] and a[3] < b[4] and b[3] < a[4]

    def _touch(self, op, reg, write):
        name = reg[0]
        b0 = reg[3] // self.BIN
        b1 = (reg[4] - 1) // self.BIN
        for b in range(b0, b1 + 1):
            key = (name, b)
            lst = self.bins.setdefault(key, [])
            newl = []
            for rec in lst:
                rect, wop, rops = rec
                if self._ovl(rect, reg):
                    if wop is not None:
                        op.deps.append(wop)
                    if write:
                        for r in rops.values():
                            op.deps.append(r)
                        lo = max(rect[3], b * self.BIN)
                        hi = min(rect[4], (b + 1) * self.BIN)
                        if reg[1] <= rect[1] and reg[2] >= rect[2] and reg[3] <= lo and reg[4] >= hi:
                            continue
                newl.append(rec)
            found = None
            for rec in newl:
                if rec[0] == reg:
                    found = rec
                    break
            if write:
                if found is not None:
                    found[1] = op
                    found[2] = {}
                else:
                    newl.append([reg, op, {}])
            else:
                if found is not None:
                    found[2][op.eng if not op.dma else ("dma", id(op))] = op
                else:
                    newl.append([reg, None, {(op.eng if not op.dma else ("dma", id(op))): op}])
            self.bins[key] = newl

    def add(self, eng, fn, reads=(), writes=(), dma=False, dram_r=(), dram_w=()):
        op = _Op(eng, fn, dma)
        op.gidx = len(self.ops)
        op.idx = len(self.per[eng])
        for ap in reads:
            rg = _region(ap)
            self._touch(op, rg, rg[0] == "ps")
        for ap in writes:
            self._touch(op, _region(ap), True)
        for nm in dram_r:
            ent = self.dram_last.setdefault(nm, [None, []])
            if ent[0] is not None:
                op.deps.append(ent[0])
            ent[1].append(op)
        for nm in dram_w:
            ent = self.dram_last.setdefault(nm, [None, []])
            if ent[0] is not None:
                op.deps.append(ent[0])
            op.deps.extend(ent[1])
            ent[0] = op
            ent[1] = []
        self.ops.append(op)
        self.per[eng].append(op)
        return op

    def finalize(self, stack):
        nc = self.nc
        dma_count = {e: 0 for e in self.ENG}
        for op in self.ops:
            if op.dma:
                i = dma_count[op.eng]
                dma_count[op.eng] += 1
                ns = self.nslot(op.eng)
                op.dma_slot = (op.eng, i % ns)
                op.dma_val = 16 * (i // ns + 1)
        dma_ops = {e: [o for o in self.per[e] if o.dma] for e in self.ENG}
        waited = {e: {f: -1 for f in self.ENG} for e in self.ENG}
        waited_dma = {e: set() for e in self.ENG}
        dma_seen = {e: 0 for e in self.ENG}
        for op in self.ops:
            e = op.eng
            need = {}
            dneed = []
            if op.dma:
                i = dma_seen[e]
                dma_seen[e] += 1
                if i >= self.nslot(e):
                    prev = dma_ops[e][i - self.nslot(e)]
                    dneed.append(prev)
            for d in op.deps:
                if d is op:
                    continue
                if d.dma:
                    dneed.append(d)
                    continue
                f = d.eng
                if f == e:
                    if e == "pe" and not op.dma:
                        continue
                    if (not op.dma) and (op.idx - d.idx) > 3:
                        continue
                    need[f] = max(need.get(f, -1), d.idx)
                else:
                    need[f] = max(need.get(f, -1), d.idx)
            for f, k in need.items():
                if k <= waited[e][f]:
                    continue
                waited[e][f] = k
                prod = self.per[f][k]
                prod.need_tok = True
                op.waits.append(prod)
            for d in dneed:
                if id(d) in waited_dma[e]:
                    continue
                waited_dma[e].add(id(d))
                op.waits.append(d)
        self.final_waits = list(self.final_dma)
        nsem = {}
        for e in self.ENG:
            k = 0
            for op in self.per[e]:
                if op.need_tok and not op.dma:
                    op.tok = (e, k // self.ROT, k % self.ROT + 1)
                    k += 1
            nsem[e] = (k + self.ROT - 1) // self.ROT
        sems = {}
        for e in self.ENG:
            for j in range(nsem[e]):
                sems[(e, j)] = stack.enter_context(nc.semaphore("s_%s_%d" % (e, j)))
            if dma_count[e]:
                for j in range(self.NSLOT):
                    sems[("dma", e, j)] = stack.enter_context(nc.semaphore("d_%s_%d" % (e, j)))
        self.sems = sems

        def emit(ename, eng):
            for op in self.per[ename]:
                for w in op.waits:
                    if w.dma:
                        eng.wait_ge(sems[("dma", w.dma_slot[0], w.dma_slot[1])], w.dma_val)
                    else:
                        eng.wait_ge(sems[(w.tok[0], w.tok[1])], w.tok[2])
                ins = op.fn(eng)
                if op.dma:
                    ins.then_inc(sems[("dma", op.dma_slot[0], op.dma_slot[1])], 16)
                elif op.tok is not None:
                    ins.then_inc(sems[(op.tok[0], op.tok[1])], 1)
            if ename == "sp":
                for w in self.final_waits:
                    eng.wait_ge(sems[("dma", w.dma_slot[0], w.dma_slot[1])], w.dma_val)

        with nc.Block() as block:
            @block.tensor
            def _(eng):
                emit("pe", eng)

            @block.scalar
            def _(eng):
                emit("act", eng)

            @block.vector
            def _(eng):
                emit("dve", eng)

            @block.gpsimd
            def _(eng):
                emit("pool", eng)

            @block.sync
            def _(eng):
                emit("sp", eng)

    def mm(self, out, lhsT, rhs, start=True, stop=True):
        rd = [lhsT, rhs] + ([] if start else [out])
        st = getattr(self, "stats", None)
        if st is None:
            st = self.stats = {}
        ph = getattr(self, "phase", "?")
        n = 1
        for d_ in rhs.shape[1:]:
            n *= int(d_)
        mul = 4 if rhs.dtype == F32 else 1
        st[ph] = st.get(ph, 0) + max(n, 64) * mul / 2.4e3
        return self.add("pe", lambda e: e.matmul(out, lhsT, rhs, start=start, stop=stop), rd, [out])

    def tr(self, out, in_, ident):
        return self.add("pe", lambda e: e.transpose(out, in_, ident), [in_, ident], [out])

    def act(self, out, in_, func, bias=None, scale=None, accum_out=None):
        kw = {}
        rd = [in_]
        if bias is not None:
            kw["bias"] = bias
            if not isinstance(bias, (int, float)):
                rd.append(bias)
        if scale is not None:
            kw["scale"] = scale
            if not isinstance(scale, (int, float)):
                rd.append(scale)
        wr = [out]
        if accum_out is not None:
            kw["accum_out"] = accum_out
            wr.append(accum_out)
        return self.add("act", lambda e: e.activation(out, in_, func, **kw), rd, wr)

    def tt(self, out, in0, in1, op, eng="dve"):
        return self.add(eng, lambda e: e.tensor_tensor(out, in0, in1, op), [in0, in1], [out])

    def ts(self, out, in0, s1, s2, op0, op1=None, eng="dve"):
        rd = [in0]
        for s in (s1, s2):
            if s is not None and not isinstance(s, (int, float)):
                rd.append(s)
        if op1 is None:
            return self.add(eng, lambda e: e.tensor_scalar(out, in0, s1, None, op0), rd, [out])
        return self.add(eng, lambda e: e.tensor_scalar(out, in0, s1, s2, op0, op1), rd, [out])

    def stt(self, out, in0, scalar, in1, op0, op1):
        rd = [in0, in1]
        if not isinstance(scalar, (int, float)):
            rd.append(scalar)
        return self.add("dve", lambda e: e.scalar_tensor_tensor(out, in0, scalar, in1, op0, op1), rd, [out])

    def copy(self, out, in_, eng="dve"):
        if eng == "act":
            return self.add("act", lambda e: e.copy(out, in_), [in_], [out])
        return self.add(eng, lambda e: e.tensor_copy(out, in_), [in_], [out])

    def recip(self, out, in_):
        return self.add("dve", lambda e: e.reciprocal(out, in_), [in_], [out])

    def recipf(self, out, in_):
        return self.add("dve", lambda e: e.reciprocal_approx_fast(out, in_), [in_], [out])

    def memset(self, ap, val, eng="dve"):
        return self.add(eng, lambda e: e.memset(ap, val), [], [ap])

    def scan(self, out, d0, d1, initial, op0, op1):
        rd = [d0, d1]
        if not isinstance(initial, (int, float)):
            rd.append(initial)
        return self.add("dve", lambda e: e.tensor_tensor_scan(out, d0, d1, initial, op0, op1), rd, [out])

    def reduce(self, out, in_, op, axis=mybir.AxisListType.X):
        return self.add("dve", lambda e: e.tensor_reduce(out, in_, axis, op), [in_], [out])

    def dma(self, out, in_, queue="sp", dram_r=(), dram_w=(), final=False):
        rd = [] if in_.tensor.name.startswith("D_") else [in_]
        wr = [] if out.tensor.name.startswith("D_") else [out]
        op = self.add(queue, lambda e: e.dma_start(out=out, in_=in_), rd, wr, dma=True, dram_r=dram_r, dram_w=dram_w)
        if final:
            self.final_dma.append(op)
        return op


class Alloc:
    def __init__(self, big, nbytes):
        self.big = big
        self.n = nbytes
        self.top = 0
        self.peak = 0

    def mark(self):
        return self.top

    def release(self, m):
        self.top = m

    def view_at(self, a, free_elems, dtype, parts=128, shape=None):
        return self._view(a, free_elems, dtype, parts, shape)

    def get(self, free_elems, dtype, parts=128, shape=None):
        sz = _dsz(dtype)
        nb = (free_elems * sz + 63) // 64 * 64
        a = self.top
        assert a + nb <= self.n, "SBUF overflow: want %d at %d (cap %d)" % (nb, a, self.n)
        self.top = a + nb
        self.peak = max(self.peak, self.top)
        return self._view(a, free_elems, dtype, parts, shape)

    def _view(self, a, free_elems, dtype, parts, shape):
        sz = _dsz(dtype)
        v = self.big[0:parts, a // 2:(a + free_elems * sz) // 2]
        if dtype != BF16:
            v = v.bitcast(dtype)
        if shape is not None:
            names = " ".join("d%d" % i for i in range(len(shape)))
            kw = {"d%d" % i: int(s) for i, s in enumerate(shape)}
            v = v.rearrange("p (%s) -> p %s" % (names, names), **kw)
        return v


class PsumPool:
    def __init__(self, ps):
        self.ps = ps
        self.i = 0

    def hold(self, dtype=F32, parts=128):
        if not hasattr(self, "held"):
            self.held = set()
        v = self.bank(dtype, parts)
        self.held.add(self.last)
        return v, self.last

    def free(self, idx):
        self.held.discard(idx)

    def bank(self, dtype=F32, parts=128):
        held = getattr(self, "held", set())
        while (self.i % 8) in held:
            self.i += 1
        b = self.i % 8
        self.last = b
        self.i += 1
        v = self.ps[0:parts, b * 512:(b + 1) * 512]
        if dtype != F32:
            v = v.bitcast(dtype)
        return v


SBUF_BYTES = 212736


class _Stop(Exception):
    pass


def build_nc(stage=99):
    from contextlib import ExitStack
    nc = bass.Bass("TRN2", target_bir_lowering=False)

    def din(name, shape, d=F32):
        return nc.dram_tensor("D_" + name, list(shape), d, kind="ExternalInput").ap()

    x_own = din("x_own", [T, D])
    x_ctx = din("x_ctx", [T, D])
    cT_d = din("cT", [128, KC])
    pos_d = din("pos", [1, 2 * T], I32)
    flag_d = din("flag", [128, 1])
    smallp_d = din("smallp", [128, 256])
    consts_d = din("consts", [128, 2048])
    w_ada = din("w_ada", [D, 6 * D])
    b_ada = din("b_ada", [1, 6 * D])
    w_in = din("w_in", [D, NCOL2])
    w_uq = din("w_uq", [512, 1536])
    w_ukv = din("w_ukv", [256, 2048])
    w_uqs = din("w_uqs", [512, 512])
    w_pab = din("w_pab", [1024, 2 * D])
    w_out = din("w_out", [D, D])
    w_rt = din("w_rt", [D, 36])
    b_rt = din("b_rt", [1, 36])
    ne_decl = N_EXP if stage >= 10 else 1
    w_ge = din("w_gate_e", [ne_decl, D, DFF])
    w_ue = din("w_up_e", [ne_decl, D, DFF])
    w_de = din("w_down_e", [ne_decl, DFF, D])
    out_d = nc.dram_tensor("D_out", [T, D], F32, kind="ExternalOutput").ap()
    xmid_d = nc.dram_tensor("D_xmid", [T, D], F32, kind="Internal").ap()
    gt_d = nc.dram_tensor("D_gt", [1, 2 * D], F32, kind="Internal").ap()

    stack = ExitStack()
    with stack:
        big = stack.enter_context(nc.sbuf_tensor("big", [128, SBUF_BYTES // 2], BF16))
        pst = stack.enter_context(nc.psum_tensor("ps", [128, 4096], F32))
        P = Prog(nc)
        A = Alloc(big, SBUF_BYTES)
        PS = PsumPool(pst)

        def ck(n, aps):
            if stage != n:
                return
            for i, ap in enumerate(aps):
                p, n_ = int(ap.shape[0]), int(ap.shape[1])
                if n_ == 1:
                    stg = A.get(2, F32, parts=p)
                    P.copy(stg[:, 0:1], ap)
                    P.copy(stg[:, 1:2], ap)
                    n_ = 2
                else:
                    stg = A.get(n_, F32, parts=p)
                    P.copy(stg, ap)
                P.dma(out_d[i * 128:i * 128 + p, 0:n_], stg, final=True)
            raise _Stop()

        def kview(w, c0, c1):
            return w.rearrange("(kc p) n -> p kc n", p=128)[:, :, c0:c1]

        def _body():
            P.phase = "const"
            ident_f = A.get(128, F32)
            P.dma(ident_f, consts_d[:, 0:128])
            ident_b = A.get(128, BF16)
            P.dma(ident_b, consts_d[:, 0:128], queue="pool")
            ones_b = A.get(128, BF16)
            P.dma(ones_b, consts_d[:, 128:256], queue="pool")
            ones_f = A.get(128, F32)
            P.dma(ones_f, consts_d[:, 128:256])
            mlmask = A.get(128, F32)
            P.dma(mlmask, consts_d[:, 256:384])
            dmask = A.get(64, F32, shape=[8, 8])
            P.dma(dmask, consts_d[:, 384:448].rearrange("p (a b) -> p a b", a=8))
            sel = A.get(8 * 128, F32, shape=[8, 128])
            P.dma(sel, consts_d[:, 448:1472].rearrange("p (a b) -> p a b", a=8))
            cv10 = A.get(10, F32)
            P.dma(cv10, consts_d[:, 1472:1482])
            invf = cv10[:, 0:1]
            sgn = cv10[:, 1:2]
            cvals = cv10[:, 2:10]
            C_EPS, C_LNK, C_ONE, C_ZERO, C_HPI = (cvals[:, i:i + 1] for i in range(5))
            maskA = A.get(4 * 512, BF16, shape=[4, 512])
            consts2_d = din("maskA", [128, 2048])
            P.dma(maskA, consts2_d.rearrange("p (a b) -> p a b", a=4), queue="pool")
            flag = A.get(1, F32)
            P.dma(flag, flag_d)
            smallp = A.get(256, F32)
            P.dma(smallp, smallp_d)
            SP_GMIX, SP_GFFN, SP_GQA, SP_GKVA, SP_CONVW, SP_CONVB, SP_GML = 0, 16, 32, 36, 38, 102, 118
            SP_GQN, SP_GQR, SP_GQRS, SP_GKN, SP_GKR, SP_GKRS, SP_BI, SP_BF = 126, 127, 128, 129, 130, 131, 132, 133
            flagones = A.get(128, BF16)
            P.ts(flagones, ones_f, flag, None, ALU.mult)
            negbig = A.get(1, F32)
            P.ts(negbig, flag, -1.0, 1.0e30, ALU.add, ALU.mult)

            modT = A.get(96, F32)
            gsc_a = A.get(KC, F32)
            gsc_m = A.get(KC, F32)
            hT = A.get(KC * T, BF16, shape=[KC, T])
            comb = A.get(8 * 32, F32, shape=[8, 32])
            base_mark = A.mark()
            hmT = A.get(H * T, BF16, shape=[H, T])
            oaT = A.get(H * T, BF16, shape=[H, T])
            r1_off = A.mark()
            state = A.get(H * 256, F32, shape=[H, 256])
            state_b = A.get(H * 256, BF16, shape=[H, 256])
            P.memset(state, 0.0, eng="pool")
            P.memset(state_b, 0.0, eng="pool")
            ckvnT = A.get(2 * 2 * T, BF16, shape=[2, 2 * T])
            krT = A.get(2 * T, BF16, parts=64)
            kpesq = A.get(2 * T, BF16, parts=64)
            cosT = A.get(2 * T, BF16, parts=64)
            sinT = A.get(2 * T, BF16, parts=64)
            qhalo = A.get(H * 3, F32, shape=[H, 3])
            khalo = A.get(H * 3, F32, shape=[H, 3])
            carry = A.get(4, F32, parts=8)
            P.memset(carry, 0.0, eng="pool")

            cT = A.get(KC, F32)
            P.dma(cT, cT_d)
            cond = A.get(KC, BF16)
            P.act(cond, cT, AF.Silu)
            P.phase = "rope"
            m2 = A.mark()
            posi = A.get(2 * T, I32, parts=64)
            P.dma(posi, pos_d[0:1, :].partition_broadcast(64) if False else pos_d.to_broadcast([64, 2 * T]))
            ang = A.get(2 * T, F32, parts=64)
            P.copy(ang, posi)
            P.ts(ang, ang, invf[0:64, :], None, ALU.mult)
            for (dst, shift, sg) in ((sinT, 0.0, True), (cosT, math.pi / 2, False)):
                m2b = A.mark()
                y = A.get(2 * T, F32, parts=64)
                kf = A.get(2 * T, F32, parts=64)
                ki = A.get(2 * T, I32, parts=64)
                P.ts(y, ang, shift, None, ALU.add)
                P.ts(kf, y, 1.0 / (2 * math.pi), None, ALU.mult)
                P.copy(ki, kf)
                P.copy(kf, ki)
                c1, c2, c3 = 6.28125, 1.9350051879882812e-3, 3.0199160695e-7
                P.stt(y, kf, -c1, y, ALU.mult, ALU.add)
                P.stt(y, kf, -c2, y, ALU.mult, ALU.add)
                P.stt(y, kf, -c3, y, ALU.mult, ALU.add)
                wr = A.get(2 * T, F32, parts=64)
                P.ts(wr, y, math.pi, -2 * math.pi, ALU.is_gt, ALU.mult)
                P.tt(y, y, wr, ALU.add)
                P.ts(wr, y, -math.pi, 2 * math.pi, ALU.is_lt, ALU.mult)
                P.tt(y, y, wr, ALU.add)
                P.ts(y, y, 3.1415925, -3.1415925, ALU.min, ALU.max)
                if sg:
                    sf = A.get(2 * T, F32, parts=64)
                    P.act(sf, y, AF.Sin)
                    P.ts(dst, sf, sgn[0:64, :], None, ALU.mult)
                else:
                    P.act(dst, y, AF.Sin)
                A.release(m2b)
            A.release(m2)
            ck(2, [cosT, sinT])

            P.phase = "adaln"
            m1 = A.mark()
            modps, modps_i = PS.hold()
            wbufs = [t_.rearrange("p a b -> p (a b)").rearrange("p (k n) -> p k n", n=512) for t_ in (hmT, oaT)]
            rowb = [A.get(512, F32, parts=1) for _ in range(1)]
            badab = [A.get(512, F32, parts=1) for _ in range(1)]
            wsel = [0]

            gbuf = {}

            def adaln_load(g, single=False):
                wi_ = 1 if single else (wsel[0] % 2)
                wsel[0] += 1
                gbuf[g] = wi_
                P.dma(wbufs[wi_], kview(w_ada, g * 512, (g + 1) * 512), queue="pool")

            def adaln_group(g, load=True):
                ph = P.phase
                P.phase = "adaln"
                if load:
                    adaln_load(g)
                wt = wbufs[gbuf[g]]
                bb = badab[0]
                P.dma(bb, b_ada[0:1, g * 512:(g + 1) * 512])
                pr = PS.bank(parts=1)
                for kc in range(KC):
                    P.mm(pr, cond[:, kc:kc + 1], wt[:, kc, :], start=(kc == 0), stop=False)
                P.mm(pr, ones_f[0:1, 0:1], bb, start=False, stop=True)
                row = rowb[0]
                P.copy(row, pr, eng="act")
                for j in range(4):
                    c = g * 4 + j
                    P.mm(modps[:, c:c + 1], row[0:1, j * 128:(j + 1) * 128], ones_f[0:1, 0:1])
                if 8 <= g < 12 or 20 <= g < 24:
                    gg = (g - 8) if g < 12 else (g - 20 + 4)
                    P.dma(gt_d[0:1, gg * 512:(gg + 1) * 512], row, dram_w=("gt%d" % gg,))
                P.phase = ph

            bg_pending = list(range(8, 24))

            bg_loaded = []

            def bg_load(n, single=False):
                for _ in range(n):
                    if bg_pending:
                        g_ = bg_pending.pop(0)
                        adaln_load(g_, single)
                        bg_loaded.append(g_)

            def bg_compute():
                while bg_loaded:
                    adaln_group(bg_loaded.pop(0), load=False)

            for g in range(8):
                adaln_group(g)
            P.copy(modT[:, 0:32], modps[:, 0:32])
            P.stt(gsc_a, modT[:, 16:32], 1.0, smallp[:, SP_GMIX:SP_GMIX + 16], ALU.add, ALU.mult)
            sh_a = modT[:, 0:16]
            sh_m = modT[:, 48:64]

            def norm_transpose(src_dram, gsc, sh, dst_hT, xkeep=None):
                mk = A.mark()
                xb = [A.get(D, F32) for _ in range(2)]
                xs_l = [A.get(D, BF16) for _ in range(2)]
                junk_l = [A.get(D, BF16) for _ in range(2)]
                ssq_l = [A.get(1, F32) for _ in range(2)]
                rstd_l = [A.get(1, F32) for _ in range(2)]
                for tb in range(T // 128):
                    xt = xb[tb % 2]
                    xs, junk, ssq, rstd = xs_l[tb % 2], junk_l[tb % 2], ssq_l[tb % 2], rstd_l[tb % 2]
                    import os
                    ksub = int(os.environ.get("KSUB", "0"))
                    P.dma(xt, src_dram[tb * 128:(tb + 1) * 128, :])
                    if ksub == 1:
                        ck(3, [xt])
                    P.act(junk, xt, AF.Square, accum_out=ssq)
                    if ksub == 2:
                        ck(3, [ssq, junk])
                    P.act(rstd, ssq, AF.Sqrt, bias=C_EPS, scale=1.0 / D)
                    P.recip(rstd, rstd)
                    if ksub == 3:
                        ck(3, [ssq, rstd])
                    P.act(xs, xt, AF.Copy, scale=rstd)
                    if ksub == 4:
                        ck(3, [ssq, rstd, xs])
                    for half in range(2):
                        pb = PS.bank(BF16)
                        for j in range(8):
                            kc = half * 8 + j
                            P.tr(pb[:, j * 128:(j + 1) * 128], xs[:, kc * 128:(kc + 1) * 128], ident_b)
                        for j in range(8):
                            kc = half * 8 + j
                            if j % 2 == 0:
                                P.ts(dst_hT[:, kc, tb * 128:(tb + 1) * 128], pb[:, j * 128:(j + 1) * 128],
                                     gsc[:, kc:kc + 1], sh[:, kc:kc + 1], ALU.mult, ALU.add)
                            else:
                                P.act(dst_hT[:, kc, tb * 128:(tb + 1) * 128], pb[:, j * 128:(j + 1) * 128], AF.Identity,
                                      bias=sh[:, kc:kc + 1], scale=gsc[:, kc:kc + 1])
                        if ksub == 5:
                            ck(3, [dst_hT[:, 0, :]])
                A.release(mk)

            wq = [0]

            def load_w(w, c0, n, nk=KC, bufs=None):
                wt = bufs[wq[0] % len(bufs)]
                wq[0] += 1
                v = wt[:, 0:nk, 0:n]
                P.dma(v, w.rearrange("(kc p) n -> p kc n", p=128)[:, :, c0:c0 + n], queue="pool")
                return v

            def fm_proj(wt, m, rhsT, t0, n, nk=KC, m0=0):
                pb = PS.bank()
                o = pb[0:m, 0:n]
                for kc in range(nk):
                    P.mm(o, wt[:, kc, m0:m0 + m], rhsT[:, kc, t0:t0 + n], start=(kc == 0), stop=(kc == nk - 1))
                return o

            def rms_bc(sq_list, nfeat, n, scratch=None):
                pb = PS.bank()
                o = pb[:, 0:n]
                for i, sq in enumerate(sq_list):
                    k = sq.shape[0]
                    P.mm(o, ones_b[0:k, :], sq, start=(i == 0), stop=(i == len(sq_list) - 1))
                r = A.get(n, F32)
                r2 = scratch if scratch is not None else A.get(n, F32)
                P.act(r2, o, AF.Ln, bias=C_EPS, scale=1.0 / nfeat)
                P.act(r, r2, AF.Exp, scale=-0.5)
                return r

            def gate_phase(tok0, is_ctx, tabs):
                mk = A.mark()
                wg = A.get(KC * 16, BF16, shape=[KC, 16])
                P.dma(wg, kview(w_in, C_I, C_I + 16), queue="pool")
                li = A.get(T, F32, parts=8)
                sp = A.get(T, F32, parts=8)
                for tg in range(2):
                    pi = fm_proj(wg, 8, hT, tg * 512, 512, m0=0)
                    P.ts(li[:, tg * 512:(tg + 1) * 512], pi, smallp[0:8, SP_BI:SP_BI + 1], None, ALU.add)
                    pf = fm_proj(wg, 8, hT, tg * 512, 512, m0=8)
                    e = A.get(512, F32, parts=8)
                    P.ts(e, pf, smallp[0:8, SP_BF:SP_BF + 1], None, ALU.add)
                    P.act(e, e, AF.Exp, scale=-1.0)
                    P.act(sp[:, tg * 512:(tg + 1) * 512], e, AF.Ln, bias=C_ONE[0:8, :])
                if is_ctx:
                    P.ts(sp, sp, flag[0:8, :], None, ALU.mult)
                    P.ts(li, li, flag[0:8, :], negbig[0:8, :], ALU.mult, ALU.add)
                Bn = A.get(T, F32, parts=8)
                U = tabs["U"] if "U" in tabs else A.get(T, F32, parts=8)
                u = A.get(T, F32, parts=8)
                P.scan(Bn, sp, sp, carry[:, 0:1], ALU.add, ALU.max)
                P.tt(u, li, Bn, ALU.add)
                P.scan(U, u, u, carry[:, 1:2], ALU.max, ALU.max)
                Ue = tabs["Ue"]
                Up = tabs["Up"]
                P.copy(Ue, U.rearrange("p (c l) -> p c l", l=128)[:, :, 127])
                P.copy(Up[:, 0:1], carry[:, 1:2])
                P.copy(Up[:, 1:8], Ue[:, 0:7])
                gh = A.get(T, F32, parts=8)
                P.tt(gh.rearrange("p (c l) -> p c l", l=128), u.rearrange("p (c l) -> p c l", l=128),
                     Ue.unsqueeze(2).to_broadcast([8, 8, 128]), ALU.subtract)
                P.act(gh, gh, AF.Exp, bias=C_LNK[0:8, :])
                gT = tabs["gT"]
                pb = PS.bank()
                for c in range(8):
                    P.tr(pb[:, c * 8:(c + 1) * 8], gh[:, c * 128:(c + 1) * 128], ident_f[0:8, 0:8])
                P.copy(gT, pb[:, 0:64].rearrange("p (c h) -> p c h", h=8))
                dec = A.get(8, F32, parts=8)
                P.tt(dec, Up, Ue, ALU.subtract)
                P.act(dec, dec, AF.Exp)
                R = A.get(64, F32, parts=8, shape=[8, 8])
                P.tt(R, dec.unsqueeze(1).to_broadcast([8, 8, 8]), dmask[0:8], ALU.mult)
                pb2 = PS.bank()
                P.mm(pb2[:, 0:64], ones_f[0:8, :], R.rearrange("p a b -> p (a b)"))
                P.copy(tabs["dec_bc"], pb2[:, 0:64].rearrange("p (h c) -> p h c", c=8))
                if not is_ctx:
                    P.tt(tabs["a"].rearrange("p (c l) -> p c l", l=128), Up.unsqueeze(2).to_broadcast([8, 8, 128]),
                         U.rearrange("p (c l) -> p c l", l=128), ALU.subtract)
                    P.act(tabs["a"], tabs["a"], AF.Exp)
                    P.tt(tabs["emt"], Bn, U, ALU.subtract)
                    P.act(tabs["emt"], tabs["emt"], AF.Exp)
                    ul = A.get(T, F32, parts=8)
                    P.ts(ul, u, C_LNK[0:8, :], None, ALU.add)
                    pb3 = PS.bank()
                    for c in range(8):
                        P.tr(pb3[:, c * 8:(c + 1) * 8], ul[:, c * 128:(c + 1) * 128], ident_f[0:8, 0:8])
                    P.copy(tabs["uT"], pb3[:, 0:64].rearrange("p (c h) -> p c h", h=8))
                P.copy(carry[:, 0:1], Bn[:, T - 1:T])
                P.copy(carry[:, 1:2], U[:, T - 1:T])
                A.release(mk)

            def alloc_tabs(own):
                tb = {}
                for k in (("U", "a", "emt") if own else ()):
                    tb[k] = A.get(T, F32, parts=8)
                tb["Ue"] = A.get(8, F32, parts=8)
                tb["Up"] = A.get(8, F32, parts=8)
                tb["gT"] = A.get(64, F32, shape=[8, 8])
                tb["dec_bc"] = A.get(64, F32, shape=[8, 8])
                if own:
                    tb["uT"] = A.get(64, F32, shape=[8, 8])
                return tb

            def conv_silu(pre, wcol, out_b):
                mkcs = A.mark()
                acc = A.get(T, F32)
                cw = smallp[:, SP_CONVW + wcol * 4: SP_CONVW + wcol * 4 + 4]
                cb = smallp[:, SP_CONVB + wcol: SP_CONVB + wcol + 1]
                P.ts(acc, pre[:, 0:T], cw[:, 0:1], cb, ALU.mult, ALU.add)
                for j in range(1, 4):
                    P.stt(acc, pre[:, j:j + T], cw[:, j:j + 1], acc, ALU.mult, ALU.add)
                P.act(out_b, acc, AF.Silu)
                A.release(mkcs)

            def mla_kv_proj(tok_off):
                mk = A.mark()
                wb = [A.get(KC * 256, BF16, shape=[KC, 256]) for _ in range(1)]
                wkv = load_w(w_in, C_CKV, 256, bufs=wb)
                wpe = A.get(KC * 128, BF16, shape=[KC, 128])
                P.dma(wpe, kview(w_in, C_KPE, C_KPE + 128), queue="pool")
                for tg in range(2):
                    mk2 = A.mark()
                    t0 = tg * 512
                    ck = A.get(2 * 512, BF16, shape=[2, 512])
                    sq = A.get(2 * 512, BF16, shape=[2, 512])
                    for c in range(2):
                        pb = fm_proj(wkv, 128, hT, t0, 512, m0=c * 128)
                        P.copy(ck[:, c, :], pb, eng="act")
                        P.act(sq[:, c, :], pb, AF.Square)
                    r = rms_bc([sq[:, 0, :], sq[:, 1, :]], 256, 512)
                    for c in range(2):
                        P.stt(ckvnT[:, c, tok_off + t0: tok_off + t0 + 512], ck[:, c, :],
                              smallp[:, SP_GKVA + c: SP_GKVA + c + 1], r, ALU.mult, ALU.mult)
                    p1 = fm_proj(wpe, 64, hT, t0, 512, m0=0)
                    p2 = fm_proj(wpe, 64, hT, t0, 512, m0=64)
                    P.act(kpesq[:, tok_off + t0: tok_off + t0 + 512], p1, AF.Square)
                    t1 = A.get(512, F32, parts=64)
                    t2 = A.get(512, F32, parts=64)
                    P.stt(t1, p1, smallp[0:64, SP_GKR:SP_GKR + 1], cosT[:, tok_off + t0: tok_off + t0 + 512], ALU.mult, ALU.mult)
                    P.stt(t2, p2, smallp[0:64, SP_GKRS:SP_GKRS + 1], sinT[:, tok_off + t0: tok_off + t0 + 512], ALU.mult, ALU.mult)
                    P.tt(krT[:, tok_off + t0: tok_off + t0 + 512], t1, t2, ALU.add)
                    A.release(mk2)
                A.release(mk)

            def alloc_wsets(n=512):
                return [A.get(KC * n, BF16, shape=[KC, n]) for _ in range(2)]

            def load_head_w(hh, own, wsets):
                ws = wsets[hh % 2]
                n = 512 if own else 384
                P.dma(ws[:, :, 0:n], kview(w_in, C_HEAD + hh * 512, C_HEAD + hh * 512 + n), queue="pool")

            def mlstm_head(hh, tabs, own, hmT=None, wsets=None):
                mk = A.mark()
                ws_ = wsets[hh % 2]
                wk, wv, wqh = (ws_[:, :, i * 128:(i + 1) * 128] for i in range(3))
                wo = ws_[:, :, 384:512] if own else None
                kpre = A.get(3 + T + 1, BF16)
                if own:
                    P.copy(kpre[:, 0:3], khalo[:, hh, :])
                else:
                    P.memset(kpre[:, 0:3], 0.0)
                for tg in range(2):
                    pb = fm_proj(wk, 128, hT, tg * 512, 512)
                    P.copy(kpre[:, 3 + tg * 512: 3 + (tg + 1) * 512], pb, eng="act")
                if not own:
                    P.ts(khalo[:, hh, :], kpre[:, T:T + 3], flag, None, ALU.mult)
                    pq = fm_proj(wqh, 128, hT, T - 3, 3)
                    P.ts(qhalo[:, hh, :], pq, flag, None, ALU.mult)
                V = A.get(8 * 128, BF16, shape=[8, 128])
                VT = A.get(T, BF16)
                for tg in range(2):
                    pbv = fm_proj(wv, 128, hT, tg * 512, 512)
                    P.copy(VT[:, tg * 512:(tg + 1) * 512], pbv, eng="act")
                KT = A.get(T, BF16)
                conv_silu(kpre, 8 + hh, KT)
                if own:
                    qpre = A.get(3 + T + 1, BF16)
                    P.copy(qpre[:, 0:3], qhalo[:, hh, :])
                    sigo = A.get(T, BF16)
                    for tg in range(2):
                        pb = fm_proj(wqh, 128, hT, tg * 512, 512)
                        P.copy(qpre[:, 3 + tg * 512: 3 + (tg + 1) * 512], pb, eng="act")
                        pb = fm_proj(wo, 128, hT, tg * 512, 512)
                        P.act(sigo[:, tg * 512:(tg + 1) * 512], pb, AF.Sigmoid)
                    QT = A.get(T, BF16)
                    conv_silu(qpre, hh, QT)
                pbv2 = PS.bank(BF16)
                for c in range(8):
                    P.tr(pbv2[:, c * 128:(c + 1) * 128], VT[:, c * 128:(c + 1) * 128], ident_b)
                P.copy(V.rearrange("p a b -> p (a b)"), pbv2, eng="act")
                Kg = A.get(8 * 128, BF16, shape=[8, 128])
                pbk = PS.bank(BF16)
                for c in range(8):
                    P.tr(pbk[:, c * 128:(c + 1) * 128], KT[:, c * 128:(c + 1) * 128], ident_b)
                for c in range(8):
                    P.ts(Kg[:, c, :], pbk[:, c * 128:(c + 1) * 128], tabs["gT"][:, c, hh:hh + 1], None, ALU.mult)
                if own:
                    QaT = A.get(T, BF16)
                    Wx = A.get(512, F32)
                    Wt = A.get(T, BF16)
                    emt_bc = A.get(T, F32)
                    for tg in range(2):
                        sl = slice(tg * 512, (tg + 1) * 512)
                        pa = PS.bank()
                        P.mm(pa, sel[0:8, hh, :], tabs["a"][:, sl])
                        P.tt(QaT[:, sl], QT[:, sl], pa, ALU.mult)
                        pu = PS.bank()
                        P.mm(pu, sel[0:8, hh, :], tabs["U"][:, sl])
                        P.tt(Wx.rearrange("p (c l) -> p c l", l=128),
                             mlmask.unsqueeze(1).to_broadcast([128, 4, 128]),
                             pu.rearrange("p (c l) -> p c l", l=128), ALU.subtract)
                        for j in range(4):
                            c = tg * 4 + j
                            P.act(Wt[:, c * 128:(c + 1) * 128], Wx[:, j * 128:(j + 1) * 128], AF.Exp,
                                  bias=tabs["uT"][:, c, hh:hh + 1])
                        pe_ = PS.bank()
                        P.mm(pe_, sel[0:8, hh, :], tabs["emt"][:, sl])
                        P.copy(emt_bc[:, sl], pe_, eng="act")
                Sm = [A.get(128, BF16) for _ in range(8)] if own else None
                sb_all = A.get(8 * 256, BF16, shape=[8, 256]) if own else None
                if own:
                    P.copy(sb_all[:, 0, :], state_b[:, hh, :], eng="act")
                for c in range(8):
                    cs = slice(c * 128, (c + 1) * 128)
                    if own:
                        sps = PS.bank()
                        P.mm(sps[:, 0:128], KT[:, cs], QT[:, cs])
                        P.tt(Sm[c], sps[:, 0:128], Wt[:, cs], ALU.mult)
                    ups = PS.bank()
                    P.mm(ups[:, 0:128], Kg[:, c, :], V[:, c, :])
                    P.mm(ups[:, 128:256], Kg[:, c, :], ones_b)
                    P.stt(state[:, hh, :], state[:, hh, :], tabs["dec_bc"][:, hh, c:c + 1], ups[:, 0:256], ALU.mult, ALU.add)
                    if own and c < 7:
                        P.copy(sb_all[:, c + 1, :], state[:, hh, :], eng="act")
                    elif not own and c == 7:
                        P.copy(state_b[:, hh, :], state[:, hh, :], eng="act")
                if own:
                    held = []
                    for tg in range(2):
                        hps, hps_i = PS.hold()
                        dps, dps_i = PS.hold()
                        held.append((hps, hps_i, dps, dps_i))
                        for j in range(4):
                            c = tg * 4 + j
                            cs = slice(c * 128, (c + 1) * 128)
                            js = slice(j * 128, (j + 1) * 128)
                            P.mm(hps[:, js], V[:, c, :], Sm[c], start=True, stop=False)
                            P.mm(hps[:, js], sb_all[:, c, 0:128], QaT[:, cs], start=False, stop=True)
                            P.mm(dps[:, js], ones_b, Sm[c], start=True, stop=False)
                            P.mm(dps[:, js], sb_all[:, c, 128:256], QaT[:, cs], start=False, stop=True)
                    mk3 = A.mark()
                    E_ = []
                    for tg in range(2):
                        hps, hps_i, dps, dps_i = held[tg]
                        E_.append(dict(sl=slice(tg * 512, (tg + 1) * 512), hps=hps, dps=dps, hi=hps_i, di=dps_i,
                                       dm=A.get(512, F32), sq=A.get(512, BF16)))
                    for e_ in E_:
                        P.act(e_["dm"], e_["dps"], AF.Abs)
                    for e_ in E_:
                        P.tt(e_["dm"], e_["dm"], emt_bc[:, e_["sl"]], ALU.max)
                    for e_ in E_:
                        P.act(e_["dm"], e_["dm"], AF.Ln)
                    for e_ in E_:
                        P.act(e_["dm"], e_["dm"], AF.Exp, scale=-1.0)
                    for e_ in E_:
                        P.tt(e_["hps"], e_["hps"], e_["dm"], ALU.mult)
                    for e_ in E_:
                        P.act(e_["sq"], e_["hps"], AF.Square)
                    for e_ in E_:
                        e_["r"] = rms_bc([e_["sq"]], 128, 512, scratch=e_["dm"])
                    for e_ in E_:
                        P.stt(e_["r"], e_["hps"], smallp[:, SP_GML + hh: SP_GML + hh + 1], e_["r"], ALU.mult, ALU.mult)
                    for e_ in E_:
                        P.tt(hmT[:, hh, e_["sl"]], e_["r"], sigo[:, e_["sl"]], ALU.mult)
                    A.release(mk3)
                    for e_ in E_:
                        PS.free(e_["hi"])
                        PS.free(e_["di"])
                A.release(mk)

            P.phase = "ctx"
            norm_transpose(x_ctx, gsc_a, sh_a, hT)
            ck(3, [hT[:, 0, :], hT[:, 15, :]])
            mkc = A.mark()
            tabs_c = alloc_tabs(False)
            gate_phase(0, True, tabs_c)
            wsets = alloc_wsets(384)
            load_head_w(0, False, wsets)
            mla_kv_proj(0)
            ck(4, [tabs_c["gT"].rearrange("p a b -> p (a b)"), tabs_c["dec_bc"].rearrange("p a b -> p (a b)"),
                   ckvnT[:, 0, 0:T], ckvnT[:, 1, 0:T], krT[:, 0:T], kpesq[:, 0:T]])
            for hh in range(H):
                bg_load(1)
                if hh + 1 < H:
                    load_head_w(hh + 1, False, wsets)
                mlstm_head(hh, tabs_c, False, wsets=wsets)
                bg_compute()
            ck(5, [state[:, 0, :], state[:, 7, :], khalo.rearrange("p a b -> p (a b)"), qhalo.rearrange("p a b -> p (a b)")])
            A.release(mkc)

            P.phase = "own_mlstm"
            norm_transpose(x_own, gsc_a, sh_a, hT)
            mko = A.mark()
            tabs_o = alloc_tabs(True)
            wsets = alloc_wsets()
            load_head_w(0, True, wsets)
            gate_phase(T, False, tabs_o)
            for hh in range(H):
                bg_load(1, single=True)
                if hh + 1 < H:
                    load_head_w(hh + 1, True, wsets)
                mlstm_head(hh, tabs_o, True, hmT=hmT, wsets=wsets)
                bg_compute()
            assert not bg_pending and not bg_loaded
            P.copy(modT[:, 32:96], modps[:, 32:96])
            PS.free(modps_i)
            P.stt(gsc_m, modT[:, 64:80], 1.0, smallp[:, SP_GFFN:SP_GFFN + 16], ALU.add, ALU.mult)
            ck(6, [hmT[:, 0, :], hmT[:, 7, :], state[:, 0, :]])
            A.release(mko)

            P.phase = "mla"
            mka = A.mark()
            mla_kv_proj(T)
            cqnT = A.get(4 * T, BF16, shape=[4, T])
            mkq = A.mark()
            wcq = A.get(KC * 512, BF16, shape=[KC, 512])
            P.dma(wcq, kview(w_in, C_CQ, C_CQ + 512), queue="pool")
            for tg in range(2):
                mk2 = A.mark()
                t0 = tg * 512
                cq = A.get(4 * 512, BF16, shape=[4, 512])
                sq = A.get(4 * 512, BF16, shape=[4, 512])
                for c in range(4):
                    pb = fm_proj(wcq, 128, hT, t0, 512, m0=c * 128)
                    P.copy(cq[:, c, :], pb, eng="act")
                    P.act(sq[:, c, :], pb, AF.Square)
                r = rms_bc([sq[:, c, :] for c in range(4)], 512, 512)
                for c in range(4):
                    P.stt(cqnT[:, c, t0:t0 + 512], cq[:, c, :], smallp[:, SP_GQA + c: SP_GQA + c + 1], r, ALU.mult, ALU.mult)
                A.release(mk2)
            A.release(mkq)
            wuq = A.get(4 * 1536, BF16, shape=[4, 1536])
            P.dma(wuq, w_uq.rearrange("(kc p) n -> p kc n", p=128), queue="pool")
            wuqs = A.get(4 * 512, BF16, shape=[4, 8, 64])
            P.dma(wuqs.rearrange("p k h c -> p k (h c)"), w_uqs.rearrange("(kc p) n -> p kc n", p=128), queue="pool")
            wukv = A.get(2 * 2048, BF16, shape=[2, 2048])
            P.dma(wukv, w_ukv.rearrange("(kc p) n -> p kc n", p=128), queue="pool")
            hbufs = [dict(qnT=A.get(T, BF16), qrT=A.get(T, BF16, parts=64), knT=A.get(2 * T, BF16),
                          Vh=A.get(16 * 128, BF16, shape=[16, 128]), rk=A.get(16, F32)) for _ in range(2)]
            pT = [A.get(512, BF16) for _ in range(4)]
            rd = A.get(512, F32)
            rd2 = A.get(512, F32)
            pi_ = [0]

            def attn_prologue(hh):
                hb = hbufs[hh % 2]
                qnT, qrT, knT, Vh, rk = hb["qnT"], hb["qrT"], hb["knT"], hb["Vh"], hb["rk"]
                for tg in range(2):
                    mk2 = A.mark()
                    t0 = tg * 512
                    sl = slice(t0, t0 + 512)
                    pn = fm_proj(wuq, 128, cqnT, t0, 512, nk=4, m0=hh * 192)
                    pr = fm_proj(wuq, 64, cqnT, t0, 512, nk=4, m0=hh * 192 + 128)
                    pbs = PS.bank()
                    prs = pbs[0:64, :]
                    for kc in range(4):
                        P.mm(prs, wuqs[:, kc, hh, :], cqnT[:, kc, sl], start=(kc == 0), stop=(kc == 3))
                    sqn = A.get(512, BF16)
                    sqr = A.get(512, BF16, parts=64)
                    P.act(sqn, pn, AF.Square)
                    P.act(sqr, pr, AF.Square)
                    r = rms_bc([sqn, sqr], 192, 512)
                    P.stt(qnT[:, sl], pn, smallp[:, SP_GQN:SP_GQN + 1], r, ALU.mult, ALU.mult)
                    t1 = A.get(512, F32, parts=64)
                    t2 = A.get(512, F32, parts=64)
                    P.stt(t1, pr, smallp[0:64, SP_GQR:SP_GQR + 1], cosT[:, T + t0: T + t0 + 512], ALU.mult, ALU.mult)
                    P.stt(t2, prs, smallp[0:64, SP_GQRS:SP_GQRS + 1], sinT[:, T + t0: T + t0 + 512], ALU.mult, ALU.mult)
                    P.tt(t1, t1, t2, ALU.add)
                    P.tt(qrT[:, sl], t1, r[0:64, :], ALU.mult)
                    A.release(mk2)
                ssqk, ssqk_i = PS.hold()
                for kg in range(4):
                    mk2 = A.mark()
                    k0 = kg * 512
                    pk = fm_proj(wukv, 128, ckvnT, k0, 512, nk=2, m0=hh * 256)
                    P.act(knT[:, k0:k0 + 512], pk, AF.Copy, scale=smallp[:, SP_GKN:SP_GKN + 1])
                    ksq = A.get(512, BF16)
                    P.act(ksq, pk, AF.Square)
                    for j in range(4):
                        kb = kg * 4 + j
                        P.mm(ssqk[:, kb:kb + 1], ksq[:, j * 128:(j + 1) * 128], ones_b[:, 0:1], start=True, stop=False)
                        P.mm(ssqk[:, kb:kb + 1], kpesq[:, kb * 128:(kb + 1) * 128], ones_b[0:64, 0:1], start=False, stop=True)
                    pv = PS.bank()
                    for j in range(4):
                        kb = kg * 4 + j
                        for kc in range(2):
                            P.mm(pv[:, j * 128:(j + 1) * 128], ckvnT[:, kc, kb * 128:(kb + 1) * 128],
                                 wukv[:, kc, hh * 256 + 128: hh * 256 + 256], start=(kc == 0), stop=(kc == 1))
                    if kg < 2:
                        P.act(Vh[:, kg * 4:(kg + 1) * 4, :], pv.rearrange("p (a b) -> p a b", b=128), AF.Copy, scale=flag)
                    else:
                        P.copy(Vh[:, kg * 4:(kg + 1) * 4, :], pv.rearrange("p (a b) -> p a b", b=128), eng="act")
                    A.release(mk2)
                P.act(rk, ssqk[:, 0:16], AF.Sqrt, bias=C_EPS, scale=1.0 / 192)
                P.recip(rk, rk)
                P.ts(rk, rk, 192 ** -0.5, None, ALU.mult)
                PS.free(ssqk_i)

            def attn_main(hh):
                hb = hbufs[hh % 2]
                qnT, qrT, knT, Vh, rk = hb["qnT"], hb["qrT"], hb["knT"], hb["Vh"], hb["rk"]
                pi = pi_[0]
                for tg in range(2):
                    sl = slice(tg * 512, (tg + 1) * 512)
                    kbs = list(range(8)) + [8 + j for j in range(4 * (tg + 1))]
                    nps, nps_i = PS.hold()
                    dps, dps_i = PS.hold()
                    pend = []
                    for i in range(len(kbs) + 2):
                        if i < len(kbs):
                            kb = kbs[i]
                            ks = slice(kb * 128, (kb + 1) * 128)
                            sps = PS.bank()
                            P.mm(sps, knT[:, ks], qnT[:, sl], start=True, stop=False)
                            P.mm(sps, krT[:, ks], qrT[:, sl], start=False, stop=True)
                            pt = pT[pi % 4]
                            pi += 1
                            P.act(pt, sps, AF.Exp, scale=rk[:, kb:kb + 1])
                            jd = kb - 8 - 4 * tg
                            if jd >= 0:
                                P.tt(pt, pt, maskA[:, jd, :], ALU.mult)
                            pend.append((i, kb, pt))
                        if i >= 2:
                            i_, kb_, pt_ = pend.pop(0)
                            P.mm(nps, Vh[:, kb_, :], pt_, start=(i_ == 0), stop=(i_ == len(kbs) - 1))
                            P.mm(dps, flagones if kb_ < 8 else ones_b, pt_, start=(i_ == 0), stop=(i_ == len(kbs) - 1))
                    P.act(rd2, dps, AF.Ln)
                    P.act(rd, rd2, AF.Exp, scale=-1.0)
                    P.tt(oaT[:, hh, sl], nps, rd, ALU.mult)
                    PS.free(nps_i)
                    PS.free(dps_i)
                pi_[0] = pi

            attn_prologue(0)
            for hh in range(H):
                if hh + 1 < H:
                    attn_prologue(hh + 1)
                attn_main(hh)
            A.release(mka)

            ck(7, [oaT[:, 0, :], oaT[:, 7, :], hmT[:, 0, :], hmT[:, 7, :]])
            P.phase = "proj"
            mixT = A.view_at(r1_off, KC * T, BF16, shape=[KC, T])
            wo_g = [A.get(KC * 512, BF16, shape=[KC, 512]) for _ in range(3)]
            mkp = A.mark()
            wgb = [A.get(KC * 256, BF16, shape=[KC, 256]) for _ in range(2)]
            wpb = [A.get(8 * 256, BF16, shape=[8, 256]) for _ in range(2)]
            def load_proj_w(c):
                wg_ = wgb[c % 2]
                wp_ = wpb[c % 2]
                P.dma(wg_, kview(w_in, C_G + c * 256, C_G + (c + 1) * 256), queue="pool")
                P.dma(wp_, kview(w_pab, c * 256, (c + 1) * 256), queue="pool")

            load_proj_w(0)
            for c in range(KC):
                wg_ = wgb[c % 2]
                wp_ = wpb[c % 2]
                if c + 1 < KC:
                    load_proj_w(c + 1)
                if c in (3, 7, 11):
                    g_ = c // 4
                    P.dma(wo_g[g_], kview(w_out, g_ * 512, (g_ + 1) * 512), queue="pool")
                for tg in range(2):
                    mk2 = A.mark()
                    t0 = tg * 512
                    pga = fm_proj(wg_, 128, hT, t0, 512, m0=0)
                    sga = A.get(512, F32)
                    P.act(sga, pga, AF.Sigmoid)
                    ppa = fm_proj(wp_, 128, oaT, t0, 512, nk=8, m0=0)
                    ma = A.get(512, F32)
                    P.tt(ma, ppa, sga, ALU.mult)
                    pgb = fm_proj(wg_, 128, hT, t0, 512, m0=128)
                    sgb = A.get(512, F32)
                    P.act(sgb, pgb, AF.Sigmoid)
                    ppb = fm_proj(wp_, 128, hmT, t0, 512, nk=8, m0=128)
                    mb = A.get(512, F32)
                    P.tt(mb, ppb, sgb, ALU.mult)
                    P.tt(mixT[:, c, t0:t0 + 512], ma, mb, ALU.add)
                    A.release(mk2)
            A.release(mkp)

            ck(8, [mixT[:, 0, :], mixT[:, 15, :]])
            P.phase = "wout"
            mkw = A.mark()
            A2 = Alloc(big, r1_off)
            A2.top = base_mark
            gt_a_bc = A2.get(D, F32)
            P.dma(gt_a_bc, gt_d[0:1, 0:D].to_broadcast([128, D]), dram_r=("gt0", "gt1", "gt2", "gt3"))
            wo_g.append(A.get(KC * 512, BF16, shape=[KC, 512]))
            P.dma(wo_g[3], kview(w_out, 3 * 512, 4 * 512), queue="pool")
            wrt = A.get(KC * 36, BF16, shape=[KC, 36])
            P.dma(wrt, w_rt.rearrange("(kc p) n -> p kc n", p=128), queue="pool")
            ring0 = A.view_at(190464, KC * 512, BF16)
            P.dma(ring0.rearrange("p (k n) -> p k n", n=512), w_ge[0].rearrange("(kc p) n -> p kc n", p=128), queue="pool")
            brt = A.get(36, F32)
            P.dma(brt, b_rt.to_broadcast([128, 36]))
            xb = [A2.get(D, F32) for _ in range(2)]
            xs = A2.get(D, BF16)
            junk = A2.get(D, BF16)
            ssq = A.get(1, F32)
            rstd = A.get(1, F32)
            def stage_a(tb):
                ts_ = slice(tb * 128, (tb + 1) * 128)
                xt = xb[tb % 2]
                P.dma(xt, x_own[ts_, :])
                for g in range(4):
                    pb = PS.bank()
                    for kc in range(KC):
                        P.mm(pb, mixT[:, kc, ts_], wo_g[g][:, kc, :], start=(kc == 0), stop=(kc == KC - 1))
                    gs = slice(g * 512, (g + 1) * 512)
                    P.tt(pb, pb, gt_a_bc[:, gs], ALU.mult)
                    P.tt(xt[:, gs], xt[:, gs], pb, ALU.add)
                P.dma(xmid_d[ts_, :], xt, dram_w=("xmid",))

            stage_a(0)
            for tb in range(8):
                ts_ = slice(tb * 128, (tb + 1) * 128)
                xt = xb[tb % 2]
                if tb + 1 < 8:
                    stage_a(tb + 1)
                P.act(junk, xt, AF.Square, accum_out=ssq)
                P.act(rstd, ssq, AF.Sqrt, bias=C_EPS, scale=1.0 / D)
                P.recip(rstd, rstd)
                P.act(xs, xt, AF.Copy, scale=rstd)
                for half in range(2):
                    pb = PS.bank(BF16)
                    for j in range(8):
                        kc = half * 8 + j
                        P.tr(pb[:, j * 128:(j + 1) * 128], xs[:, kc * 128:(kc + 1) * 128], ident_b)
                    for j in range(8):
                        kc = half * 8 + j
                        P.ts(hT[:, kc, ts_], pb[:, j * 128:(j + 1) * 128], gsc_m[:, kc:kc + 1], sh_m[:, kc:kc + 1], ALU.mult, ALU.add)
                mk2 = A.mark()
                pl = PS.bank()
                for kc in range(KC):
                    P.mm(pl[:, 0:36], hT[:, kc, ts_], wrt[:, kc, :], start=(kc == 0), stop=(kc == KC - 1))
                lg = A.get(36, F32)
                P.tt(lg, pl[:, 0:36], brt, ALU.add)
                gl = lg[:, 0:4]
                el = lg[:, 4:36].rearrange("p (j e) -> p j e", e=8)
                gmax = A.get(1, F32)
                P.reduce(gmax, gl, ALU.max)
                ngmax = A.get(1, F32)
                P.ts(ngmax, gmax, -1.0, None, ALU.mult)
                ge = A.get(4, F32)
                gsum = A.get(1, F32)
                P.act(ge, gl, AF.Exp, bias=ngmax, accum_out=gsum)
                gw = A.get(1, F32)
                P.recip(gw, gsum)
                oh = A.get(4, F32)
                P.ts(oh, gl, gmax, None, ALU.is_equal)
                esel = A.get(32, F32, shape=[4, 8])
                P.tt(esel, el, oh.unsqueeze(2).to_broadcast([128, 4, 8]), ALU.mult)
                ein = A.get(8, F32)
                P.reduce(ein, esel.rearrange("p j e -> p e j"), ALU.add)
                l1 = A.get(1, F32)
                P.reduce(l1, ein, ALU.max)
                mk1_ = A.get(8, F32)
                P.ts(mk1_, ein, l1, None, ALU.is_equal)
                ein2 = A.get(8, F32)
                P.stt(ein2, mk1_, -1.0e30, ein, ALU.mult, ALU.add)
                l2 = A.get(1, F32)
                P.reduce(l2, ein2, ALU.max)
                mk2_ = A.get(8, F32)
                P.ts(mk2_, ein2, l2, None, ALU.is_equal)
                dd = A.get(1, F32)
                P.tt(dd, l2, l1, ALU.subtract)
                P.act(dd, dd, AF.Exp)
                rr = A.get(1, F32)
                P.ts(rr, dd, 1.0, None, ALU.add)
                P.recip(rr, rr)
                w1 = A.get(1, F32)
                P.tt(w1, gw, rr, ALU.mult)
                w2 = A.get(1, F32)
                P.tt(w2, w1, dd, ALU.mult)
                cg = A.get(8, F32)
                assert A.top <= 190464
                P.ts(cg, mk1_, w1, None, ALU.mult)
                P.stt(cg, mk2_, w2, cg, ALU.mult, ALU.add)
                P.tt(comb[:, tb, :].rearrange("p (j e) -> p j e", e=8), oh.unsqueeze(2).to_broadcast([128, 4, 8]),
                     cg.unsqueeze(1).to_broadcast([128, 4, 8]), ALU.mult)
                A.release(mk2)
            A.release(mkw)

            ck(9, [hT[:, 0, :], hT[:, 15, :], comb.rearrange("p a b -> p (a b)")])
            P.phase = "moe"
            A.release(base_mark)
            gt_m_bc = A.get(D, F32)
            P.dma(gt_m_bc, gt_d[0:1, D:2 * D].to_broadcast([128, D]), dram_r=("gt4", "gt5", "gt6", "gt7"))
            macc = A.get(8 * D, F32, shape=[8, D])
            P.memset(macc, 0.0, eng="pool")
            ring = [ring0] + [A.get(KC * 512, BF16) for _ in range(2)]
            assert A.top <= 190464
            sg = A.get(4 * T, BF16, shape=[4, T])
            ri = 0
            for e in range(N_EXP):
                wgt = ring[ri % 3].rearrange("p (k n) -> p k n", n=512)
                ri += 1
                if e > 0:
                    P.dma(wgt, w_ge[e].rearrange("(kc p) n -> p kc n", p=128), queue="pool")
                wut = ring[ri % 3].rearrange("p (k n) -> p k n", n=512)
                ri += 1
                P.dma(wut, w_ue[e].rearrange("(kc p) n -> p kc n", p=128), queue="pool")
                wdt = ring[ri % 3].rearrange("p (k n) -> p k n", n=D)
                ri += 1
                P.dma(wdt, w_de[e].rearrange("(kc p) n -> p kc n", p=128), queue="pool")
                for fc in range(4):
                    for tg in range(2):
                        pb = fm_proj(wgt, 128, hT, tg * 512, 512, m0=fc * 128)
                        P.act(sg[:, fc, tg * 512:(tg + 1) * 512], pb, AF.Silu)
                for fc in range(4):
                    for tg in range(2):
                        pb = fm_proj(wut, 128, hT, tg * 512, 512, m0=fc * 128)
                        sl = slice(tg * 512, (tg + 1) * 512)
                        P.tt(sg[:, fc, sl], pb, sg[:, fc, sl], ALU.mult)
                for tb in range(8):
                    ts_ = slice(tb * 128, (tb + 1) * 128)
                    for g in range(4):
                        pb = PS.bank()
                        for fc in range(4):
                            P.mm(pb, sg[:, fc, ts_], wdt[:, fc, g * 512:(g + 1) * 512], start=(fc == 0), stop=(fc == 3))
                        gs = slice(g * 512, (g + 1) * 512)
                        P.stt(macc[:, tb, gs], pb, comb[:, tb, e:e + 1], macc[:, tb, gs], ALU.mult, ALU.add)

            P.phase = "final"
            xo = [ring[i].bitcast(F32)[:, 0:D] for i in range(2)]
            for tb in range(8):
                ts_ = slice(tb * 128, (tb + 1) * 128)
                xt = xo[tb % 2]
                P.dma(xt, xmid_d[ts_, :], dram_r=("xmid",))
                P.tt(macc[:, tb, :], macc[:, tb, :], gt_m_bc, ALU.mult, eng="pool")
                P.tt(xt, xt, macc[:, tb, :], ALU.add)
                P.dma(out_d[ts_, :], xt, final=True)

        try:
            _body()
        except _Stop:
            pass
        P.finalize(stack)
        print("PE est us per phase:", {k: int(v) for k, v in getattr(P, "stats", {}).items()})
        print("SBUF peak bytes/partition:", A.peak, " ops:", len(P.ops), {e: len(P.per[e]) for e in P.ENG})
    return nc


def _host_consts():
    c = np.zeros((128, 2048), np.float32)
    c[:, 0:128] = np.eye(128, dtype=np.float32)
    c[:, 128:256] = 1.0
    s = np.arange(128)[:, None]
    t = np.arange(128)[None, :]
    c[:, 256:384] = np.where(s <= t, 0.0, NEG)
    dm = np.zeros((128, 8, 8), np.float32)
    for h in range(8):
        dm[h, h, :] = 1.0
    c[:, 384:448] = dm.reshape(128, 64)
    sl = np.zeros((128, 8, 128), np.float32)
    for h in range(8):
        sl[h, h, :] = 1.0
    c[:, 448:1472] = sl.reshape(128, 1024)
    inv = (10000.0 ** (-np.arange(0, 64, 2, dtype=np.float32) / 64)).astype(np.float32)
    c[0:32, 1472] = inv
    c[32:64, 1472] = inv
    c[0:32, 1473] = -1.0
    c[32:64, 1473] = 1.0
    c[:, 1474] = EPS
    c[:, 1475] = LN_KSCALE
    c[:, 1476] = 1.0
    c[:, 1477] = 0.0
    c[:, 1478] = math.pi / 2
    mA = np.zeros((128, 4, 512), np.float32)
    k = np.arange(128)[:, None]
    q = np.arange(512)[None, :]
    for j in range(4):
        mA[:, j, :] = (128 * j + k <= q)
    return c, mA.reshape(128, 2048)


def _fm(v, nchunk):
    return np.ascontiguousarray(v.reshape(nchunk, 128).T)


_CACHE = {}


def kernel(**inp):
    f32 = np.float32
    x = np.asarray(inp["x"], f32)
    c = np.asarray(inp["c"], f32)
    pos = np.asarray(inp["positions"], np.int32)
    consts, maskA = _host_consts()
    sp = np.zeros((128, 256), f32)
    sp[:, 0:16] = _fm(np.asarray(inp["norm_mix_g"], f32)[0], 16)
    sp[:, 16:32] = _fm(np.asarray(inp["norm_ffn_g"], f32)[0], 16)
    sp[:, 32:36] = _fm(np.asarray(inp["q_a_norm_g"], f32)[0], 4)
    sp[:, 36:38] = _fm(np.asarray(inp["kv_a_norm_g"], f32)[0], 2)
    cw = np.asarray(inp["conv_w"], f32)[0]
    sp[:, 38:102] = cw.reshape(4, 16, 128).transpose(2, 1, 0).reshape(128, 64)
    sp[:, 102:118] = _fm(np.asarray(inp["conv_b"], f32)[0], 16)
    sp[:, 118:126] = _fm(np.asarray(inp["mlstm_norm_g"], f32)[0], 8)
    gq = np.asarray(inp["q_norm_g"], f32)[0]
    gk = np.asarray(inp["k_norm_g"], f32)[0]
    sp[:, 126] = gq[0:128]
    sp[0:64, 127] = gq[128:192]
    sp[0:64, 128] = np.concatenate([gq[160:192], gq[128:160]])
    sp[:, 129] = gk[0:128]
    sp[0:64, 130] = gk[128:192]
    sp[0:64, 131] = np.concatenate([gk[160:192], gk[128:160]])
    bg = np.asarray(inp["b_mlstm_gates"], f32)[0]
    sp[0:8, 132] = bg[0]
    sp[0:8, 133] = bg[1]
    w_rt = np.ascontiguousarray(np.concatenate([np.asarray(inp["w_group"], f32)[0], np.asarray(inp["w_router"], f32)[0]], axis=1))
    b_rt = np.ascontiguousarray(np.concatenate([np.asarray(inp["b_group"], f32)[0], np.asarray(inp["b_router"], f32)[0]])[None, :])
    import os
    wi = np.asarray(inp["w_in"], f32)[0]
    cols = []
    for h in range(8):
        cols += [wi[:, 1856 + h * 128:1856 + (h + 1) * 128], wi[:, 2880 + h * 128:2880 + (h + 1) * 128],
                 wi[:, 832 + h * 128:832 + (h + 1) * 128], wi[:, 3904 + h * 128:3904 + (h + 1) * 128]]
    for c_ in range(16):
        cols += [wi[:, 4944 + c_ * 128:4944 + (c_ + 1) * 128], wi[:, 6992 + c_ * 128:6992 + (c_ + 1) * 128]]
    cols += [wi[:, 0:512], wi[:, 512:768], wi[:, 768:832], wi[:, 800:832], wi[:, 768:800], wi[:, 4928:4944]]
    w_in2 = np.ascontiguousarray(np.concatenate(cols, axis=1))
    assert w_in2.shape == (D, NCOL2)
    wpa = np.asarray(inp["w_proj_a"], f32)[0]
    wpb_ = np.asarray(inp["w_proj_b"], f32)[0]
    w_pab = np.ascontiguousarray(np.concatenate(
        [np.concatenate([wpa[:, c_ * 128:(c_ + 1) * 128], wpb_[:, c_ * 128:(c_ + 1) * 128]], axis=1) for c_ in range(16)], axis=1))
    wq_ = np.asarray(inp["w_uq"], f32)[0]
    w_uqs = np.ascontiguousarray(np.concatenate(
        [np.concatenate([wq_[:, h * 192 + 160:h * 192 + 192], wq_[:, h * 192 + 128:h * 192 + 160]], axis=1) for h in range(8)], axis=1))
    stage = int(os.environ.get("KSTAGE", "99"))
    ne_decl = N_EXP if stage >= 10 else 1
    shared = {
        "D_consts": consts, "D_maskA": maskA, "D_smallp": sp,
        "D_w_ada": np.asarray(inp["w_ada"], f32)[0], "D_b_ada": np.asarray(inp["b_ada"], f32),
        "D_w_in": w_in2, "D_w_uq": np.asarray(inp["w_uq"], f32)[0],
        "D_w_ukv": np.asarray(inp["w_ukv"], f32)[0], "D_w_pab": w_pab, "D_w_uqs": w_uqs, "D_w_out": np.asarray(inp["w_out"], f32)[0],
        "D_w_rt": w_rt, "D_b_rt": b_rt,
        "D_w_gate_e": np.asarray(inp["w_gate_e"], f32)[0][:ne_decl], "D_w_up_e": np.asarray(inp["w_up_e"], f32)[0][:ne_decl],
        "D_w_down_e": np.asarray(inp["w_down_e"], f32)[0][:ne_decl],
    }
    in_maps = []
    for core in range(8):
        b, hf = core // 2, core % 2
        m = dict(shared)
        m["D_x_own"] = np.ascontiguousarray(x[b, hf * T:(hf + 1) * T])
        m["D_x_ctx"] = np.ascontiguousarray(x[b, 0:T])
        m["D_cT"] = _fm(c[b], 16)
        m["D_pos"] = np.ascontiguousarray(np.concatenate([pos[b, 0:T], pos[b, hf * T:(hf + 1) * T]])[None, :])
        m["D_flag"] = np.full((128, 1), float(hf), f32)
        in_maps.append(m)
    if "nc" not in _CACHE:
        _CACHE["nc"] = build_nc(stage)
    ncores = int(os.environ.get("KCORES", "8"))
    res = run_bass_kernel_spmd(_CACHE["nc"], in_maps[:ncores], core_ids=list(range(ncores)))
    out = np.zeros((NB, S, D), f32)
    for core in range(ncores):
        b, hf = core // 2, core % 2
        out[b, hf * T:(hf + 1) * T] = np.asarray(res.results[core]["D_out"], f32)
    return out
```

```python
import math
import numpy as np
import concourse.bass as bass
import concourse.mybir as mybir
from concourse.bass_utils import run_bass_kernel_spmd
from concourse.alu_op_type import AluOpType as ALU

dt = mybir.dt
AF = mybir.ActivationFunctionType
F32 = dt.float32
BF16 = dt.bfloat16
I32 = dt.int32

D = 2048
S = 2048
NB = 4
T = 1024
KC = 16
H = 8
EPS = 1e-6
N_EXP = 32
DFF = 512
C_HEAD, C_G, C_CQ, C_CKV, C_KPE, C_KPES, C_I = 0, 4096, 8192, 8704, 8960, 9024, 9088
NCOL2 = 9104
LN_KSCALE = math.log(128 ** -0.5)
NEG = -30000.0

_DSIZE = {F32: 4, BF16: 2, I32: 4}


def _dsz(d):
    return _DSIZE[d]


class _Op:
    __slots__ = ("eng", "fn", "idx", "gidx", "dma", "deps", "tok", "need_tok", "dma_slot", "dma_val", "waits")

    def __init__(self, eng, fn, dma):
        self.eng = eng
        self.fn = fn
        self.dma = dma
        self.deps = []
        self.tok = None
        self.need_tok = False
        self.dma_slot = None
        self.dma_val = 0
        self.waits = []


def _region(ap):
    t = ap.tensor
    name = t.name
    shape = list(t.shape)
    dsz = _dsz(ap.dtype)
    row = 1
    for s in shape[1:]:
        row *= int(s)
    off = int(ap.offset)
    apl = [(int(a), int(b)) for a, b in ap.ap]
    pstep, pcnt = apl[0]
    p0 = off // row
    f0 = off % row
    ext = 1
    for s, c in apl[1:]:
        ext += (c - 1) * abs(s)
    p1 = p0 + (pcnt if pstep != 0 else 1)
    if name == "ps":
        bk = (f0 * dsz) // 2048
        return (name, 0, 128, bk * 2048, (bk + 1) * 2048)
    return (name, p0, p1, f0 * dsz, (f0 + ext) * dsz)


class Prog:
    ENG = ("pe", "act", "dve", "pool", "sp")
    ROT = 1500
    NSLOT = 8
    BIN = 2048

    def nslot(self, e):
        return 2 if e == "pool" else self.NSLOT

    def __init__(self, nc):
        self.nc = nc
        self.ops = []
        self.per = {e: [] for e in self.ENG}
        self.bins = {}
        self.dram_last = {}
        self.final_dma = []

    @staticmethod
    def _ovl(a, b):
        return a[1] < b[2] and b[1] < a[2] and a[3] < b[4] and b[3] < a[4]

    def _touch(self, op, reg, write):
        name = reg[0]
        b0 = reg[3] // self.BIN
        b1 = (reg[4] - 1) // self.BIN
        for b in range(b0, b1 + 1):
            key = (name, b)
            lst = self.bins.setdefault(key, [])
            newl = []
            for rec in lst:
                rect, wop, rops = rec
                if self._ovl(rect, reg):
                    if wop is not None:
                        op.deps.append(wop)
                    if write:
                        for r in rops.values():
                            op.deps.append(r)
                        lo = max(rect[3], b * self.BIN)
                        hi = min(rect[4], (b + 1) * self.BIN)
                        if reg[1] <= rect[1] and reg[2] >= rect[2] and reg[3] <= lo and reg[4] >= hi:
                            continue
                newl.append(rec)
            found = None
            for rec in newl:
                if rec[0] == reg:
                    found = rec
                    break
            if write:
                if found is not None:
                    found[1] = op
                    found[2] = {}
                else:
                    newl.append([reg, op, {}])
            else:
                if found is not None:
                    found[2][op.eng if not op.dma else ("dma", id(op))] = op
                else:
                    newl.append([reg, None, {(op.eng if not op.dma else ("dma", id(op))): op}])
            self.bins[key] = newl

    def add(self, eng, fn, reads=(), writes=(), dma=False, dram_r=(), dram_w=()):
        op = _Op(eng, fn, dma)
        op.gidx = len(self.ops)
        op.idx = len(self.per[eng])
        for ap in reads:
            rg = _region(ap)
            self._touch(op, rg, rg[0] == "ps")
        for ap in writes:
            self._touch(op, _region(ap), True)
        for nm in dram_r:
            ent = self.dram_last.setdefault(nm, [None, []])
            if ent[0] is not None:
                op.deps.append(ent[0])
            ent[1].append(op)
        for nm in dram_w:
            ent = self.dram_last.setdefault(nm, [None, []])
            if ent[0] is not None:
                op.deps.append(ent[0])
            op.deps.extend(ent[1])
            ent[0] = op
            ent[1] = []
        self.ops.append(op)
        self.per[eng].append(op)
        return op

    def finalize(self, stack):
        nc = self.nc
        dma_count = {e: 0 for e in self.ENG}
        for op in self.ops:
            if op.dma:
                i = dma_count[op.eng]
                dma_count[op.eng] += 1
                ns = self.nslot(op.eng)
                op.dma_slot = (op.eng, i % ns)
                op.dma_val = 16 * (i // ns + 1)
        dma_ops = {e: [o for o in self.per[e] if o.dma] for e in self.ENG}
        waited = {e: {f: -1 for f in self.ENG} for e in self.ENG}
        waited_dma = {e: set() for e in self.ENG}
        dma_seen = {e: 0 for e in self.ENG}
        for op in self.ops:
            e = op.eng
            need = {}
            dneed = []
            if op.dma:
                i = dma_seen[e]
                dma_seen[e] += 1
                if i >= self.nslot(e):
                    prev = dma_ops[e][i - self.nslot(e)]
                    dneed.append(prev)
            for d in op.deps:
                if d is op:
                    continue
                if d.dma:
                    dneed.append(d)
                    continue
                f = d.eng
                if f == e:
                    if e == "pe" and not op.dma:
                        continue
                    if (not op.dma) and (op.idx - d.idx) > 3:
                        continue
                    need[f] = max(need.get(f, -1), d.idx)
                else:
                    need[f] = max(need.get(f, -1), d.idx)
            for f, k in need.items():
                if k <= waited[e][f]:
                    continue
                waited[e][f] = k
                prod = self.per[f][k]
                prod.need_tok = True
                op.waits.append(prod)
            for d in dneed:
                if id(d) in waited_dma[e]:
                    continue
                waited_dma[e].add(id(d))
                op.waits.append(d)
        self.final_waits = list(self.final_dma)
        nsem = {}
        for e in self.ENG:
            k = 0
            for op in self.per[e]:
                if op.need_tok and not op.dma:
                    op.tok = (e, k // self.ROT, k % self.ROT + 1)
                    k += 1
            nsem[e] = (k + self.ROT - 1) // self.ROT
        sems = {}
        for e in self.ENG:
            for j in range(nsem[e]):
                sems[(e, j)] = stack.enter_context(nc.semaphore("s_%s_%d" % (e, j)))
            if dma_count[e]:
                for j in range(self.NSLOT):
                    sems[("dma", e, j)] = stack.enter_context(nc.semaphore("d_%s_%d" % (e, j)))
        self.sems = sems

        def emit(ename, eng):
            for op in self.per[ename]:
                for w in op.waits:
                    if w.dma:
                        eng.wait_ge(sems[("dma", w.dma_slot[0], w.dma_slot[1])], w.dma_val)
                    else:
                        eng.wait_ge(sems[(w.tok[0], w.tok[1])], w.tok[2])
                ins = op.fn(eng)
                if op.dma:
                    ins.then_inc(sems[("dma", op.dma_slot[0], op.dma_slot[1])], 16)
                elif op.tok is not None:
                    ins.then_inc(sems[(op.tok[0], op.tok[1])], 1)
            if ename == "sp":
                for w in self.final_waits:
                    eng.wait_ge(sems[("dma", w.dma_slot[0], w.dma_slot[1])], w.dma_val)

        with nc.Block() as block:
            @block.tensor
            def _(eng):
                emit("pe", eng)

            @block.scalar
            def _(eng):
                emit("act", eng)

            @block.vector
            def _(eng):
                emit("dve", eng)

            @block.gpsimd
            def _(eng):
                emit("pool", eng)

            @block.sync
            def _(eng):
                emit("sp", eng)

    def mm(self, out, lhsT, rhs, start=True, stop=True):
        rd = [lhsT, rhs] + ([] if start else [out])
        st = getattr(self, "stats", None)
        if st is None:
            st = self.stats = {}
        ph = getattr(self, "phase", "?")
        n = 1
        for d_ in rhs.shape[1:]:
            n *= int(d_)
        mul = 4 if rhs.dtype == F32 else 1
        st[ph] = st.get(ph, 0) + max(n, 64) * mul / 2.4e3
        return self.add("pe", lambda e: e.matmul(out, lhsT, rhs, start=start, stop=stop), rd, [out])

    def tr(self, out, in_, ident):
        return self.add("pe", lambda e: e.transpose(out, in_, ident), [in_, ident], [out])

    def act(self, out, in_, func, bias=None, scale=None, accum_out=None):
        kw = {}
        rd = [in_]
        if bias is not None:
            kw["bias"] = bias
            if not isinstance(bias, (int, float)):
                rd.append(bias)
        if scale is not None:
            kw["scale"] = scale
            if not isinstance(scale, (int, float)):
                rd.append(scale)
        wr = [out]
        if accum_out is not None:
            kw["accum_out"] = accum_out
            wr.append(accum_out)
        return self.add("act", lambda e: e.activation(out, in_, func, **kw), rd, wr)

    def tt(self, out, in0, in1, op, eng="dve"):
        return self.add(eng, lambda e: e.tensor_tensor(out, in0, in1, op), [in0, in1], [out])

    def ts(self, out, in0, s1, s2, op0, op1=None, eng="dve"):
        rd = [in0]
        for s in (s1, s2):
            if s is not None and not isinstance(s, (int, float)):
                rd.append(s)
        if op1 is None:
            return self.add(eng, lambda e: e.tensor_scalar(out, in0, s1, None, op0), rd, [out])
        return self.add(eng, lambda e: e.tensor_scalar(out, in0, s1, s2, op0, op1), rd, [out])

    def stt(self, out, in0, scalar, in1, op0, op1):
        rd = [in0, in1]
        if not isinstance(scalar, (int, float)):
            rd.append(scalar)
        return self.add("dve", lambda e: e.scalar_tensor_tensor(out, in0, scalar, in1, op0, op1), rd, [out])

    def copy(self, out, in_, eng="dve"):
        if eng == "act":
            return self.add("act", lambda e: e.copy(out, in_), [in_], [out])
        return self.add(eng, lambda e: e.tensor_copy(out, in_), [in_], [out])

    def recip(self, out, in_):
        return self.add("dve", lambda e: e.reciprocal(out, in_), [in_], [out])

    def recipf(self, out, in_):
        return self.add("dve", lambda e: e.reciprocal_approx_fast(out, in_), [in_], [out])

    def memset(self, ap, val, eng="dve"):
        return self.add(eng, lambda e: e.memset(ap, val), [], [ap])

    def scan(self, out, d0, d1, initial, op0, op1):
        rd = [d0, d1]
        if not isinstance(initial, (int, float)):
            rd.append(initial)
        return self.add("dve", lambda e: e.tensor_tensor_scan(out, d0, d1, initial, op0, op1), rd, [out])

    def reduce(self, out, in_, op, axis=mybir.AxisListType.X):
        return self.add("dve", lambda e: e.tensor_reduce(out, in_, axis, op), [in_], [out])

    def dma(self, out, in_, queue="sp", dram_r=(), dram_w=(), final=False):
        rd = [] if in_.tensor.name.startswith("D_") else [in_]
        wr = [] if out.tensor.name.startswith("D_") else [out]
        op = self.add(queue, lambda e: e.dma_start(out=out, in_=in_), rd, wr, dma=True, dram_r=dram_r, dram_w=dram_w)
        if final:
            self.final_dma.append(op)
        return op


class Alloc:
    def __init__(self, big, nbytes):
        self.big = big
        self.n = nbytes
        self.top = 0
        self.peak = 0

    def mark(self):
        return self.top

    def release(self, m):
        self.top = m

    def view_at(self, a, free_elems, dtype, parts=128, shape=None):
        return self._view(a, free_elems, dtype, parts, shape)

    def get(self, free_elems, dtype, parts=128, shape=None):
        sz = _dsz(dtype)
        nb = (free_elems * sz + 63) // 64 * 64
        a = self.top
        assert a + nb <= self.n, "SBUF overflow: want %d at %d (cap %d)" % (nb, a, self.n)
        self.top = a + nb
        self.peak = max(self.peak, self.top)
        return self._view(a, free_elems, dtype, parts, shape)

    def _view(self, a, free_elems, dtype, parts, shape):
        sz = _dsz(dtype)
        v = self.big[0:parts, a // 2:(a + free_elems * sz) // 2]
        if dtype != BF16:
            v = v.bitcast(dtype)
        if shape is not None:
            names = " ".join("d%d" % i for i in range(len(shape)))
            kw = {"d%d" % i: int(s) for i, s in enumerate(shape)}
            v = v.rearrange("p (%s) -> p %s" % (names, names), **kw)
        return v


class PsumPool:
    def __init__(self, ps):
        self.ps = ps
        self.i = 0

    def hold(self, dtype=F32, parts=128):
        if not hasattr(self, "held"):
            self.held = set()
        v = self.bank(dtype, parts)
        self.held.add(self.last)
        return v, self.last

    def free(self, idx):
        self.held.discard(idx)

    def bank(self, dtype=F32, parts=128):
        held = getattr(self, "held", set())
        while (self.i % 8) in held:
            self.i += 1
        b = self.i % 8
        self.last = b
        self.i += 1
        v = self.ps[0:parts, b * 512:(b + 1) * 512]
        if dtype != F32:
            v = v.bitcast(dtype)
        return v


SBUF_BYTES = 212736


class _Stop(Exception):
    pass


def build_nc(stage=99):
    from contextlib import ExitStack
    nc = bass.Bass("TRN2", target_bir_lowering=False)

    def din(name, shape, d=F32):
        return nc.dram_tensor("D_" + name, list(shape), d, kind="ExternalInput").ap()

    x_own = din("x_own", [T, D])
    x_ctx = din("x_ctx", [T, D])
    cT_d = din("cT", [128, KC])
    pos_d = din("pos", [1, 2 * T], I32)
    flag_d = din("flag", [128, 1])
    smallp_d = din("smallp", [128, 256])
    consts_d = din("consts", [128, 2048])
    w_ada = din("w_ada", [D, 6 * D])
    b_ada = din("b_ada", [1, 6 * D])
    w_in = din("w_in", [D, NCOL2])
    w_uq = din("w_uq", [512, 1536])
    w_ukv = din("w_ukv", [256, 2048])
    w_uqs = din("w_uqs", [512, 512])
    w_pab = din("w_pab", [1024, 2 * D])
    w_out = din("w_out", [D, D])
    w_rt = din("w_rt", [D, 36])
    b_rt = din("b_rt", [1, 36])
    ne_decl = N_EXP if stage >= 10 else 1
    w_ge = din("w_gate_e", [ne_decl, D, DFF])
    w_ue = din("w_up_e", [ne_decl, D, DFF])
    w_de = din("w_down_e", [ne_decl, DFF, D])
    out_d = nc.dram_tensor("D_out", [T, D], F32, kind="ExternalOutput").ap()
    xmid_d = nc.dram_tensor("D_xmid", [T, D], F32, kind="Internal").ap()
    gt_d = nc.dram_tensor("D_gt", [1, 2 * D], F32, kind="Internal").ap()

    stack = ExitStack()
    with stack:
        big = stack.enter_context(nc.sbuf_tensor("big", [128, SBUF_BYTES // 2], BF16))
        pst = stack.enter_context(nc.psum_tensor("ps", [128, 4096], F32))
        P = Prog(nc)
        A = Alloc(big, SBUF_BYTES)
        PS = PsumPool(pst)

        def ck(n, aps):
            if stage != n:
                return
            for i, ap in enumerate(aps):
                p, n_ = int(ap.shape[0]), int(ap.shape[1])
                if n_ == 1:
                    stg = A.get(2, F32, parts=p)
                    P.copy(stg[:, 0:1], ap)
                    P.copy(stg[:, 1:2], ap)
                    n_ = 2
                else:
                    stg = A.get(n_, F32, parts=p)
                    P.copy(stg, ap)
                P.dma(out_d[i * 128:i * 128 + p, 0:n_], stg, final=True)
            raise _Stop()

        def kview(w, c0, c1):
            return w.rearrange("(kc p) n -> p kc n", p=128)[:, :, c0:c1]

        def _body():
            P.phase = "const"
            ident_f = A.get(128, F32)
            P.dma(ident_f, consts_d[:, 0:128])
            ident_b = A.get(128, BF16)
            P.dma(ident_b, consts_d[:, 0:128], queue="pool")
            ones_b = A.get(128, BF16)
            P.dma(ones_b, consts_d[:, 128:256], queue="pool")
            ones_f = A.get(128, F32)
            P.dma(ones_f, consts_d[:, 128:256])
            mlmask = A.get(128, F32)
            P.dma(mlmask, consts_d[:, 256:384])
            dmask = A.get(64, F32, shape=[8, 8])
            P.dma(dmask, consts_d[:, 384:448].rearrange("p (a b) -> p a b", a=8))
            sel = A.get(8 * 128, F32, shape=[8, 128])
            P.dma(sel, consts_d[:, 448:1472].rearrange("p (a b) -> p a b", a=8))
            cv10 = A.get(10, F32)
            P.dma(cv10, consts_d[:, 1472:1482])
            invf = cv10[:, 0:1]
            sgn = cv10[:, 1:2]
            cvals = cv10[:, 2:10]
            C_EPS, C_LNK, C_ONE, C_ZERO, C_HPI = (cvals[:, i:i + 1] for i in range(5))
            maskA = A.get(4 * 512, BF16, shape=[4, 512])
            consts2_d = din("maskA", [128, 2048])
            P.dma(maskA, consts2_d.rearrange("p (a b) -> p a b", a=4), queue="pool")
            flag = A.get(1, F32)
            P.dma(flag, flag_d)
            smallp = A.get(256, F32)
            P.dma(smallp, smallp_d)
            SP_GMIX, SP_GFFN, SP_GQA, SP_GKVA, SP_CONVW, SP_CONVB, SP_GML = 0, 16, 32, 36, 38, 102, 118
            SP_GQN, SP_GQR, SP_GQRS, SP_GKN, SP_GKR, SP_GKRS, SP_BI, SP_BF = 126, 127, 128, 129, 130, 131, 132, 133
            flagones = A.get(128, BF16)
            P.ts(flagones, ones_f, flag, None, ALU.mult)
            negbig = A.get(1, F32)
            P.ts(negbig, flag, -1.0, 1.0e30, ALU.add, ALU.mult)

            modT = A.get(96, F32)
            gsc_a = A.get(KC, F32)
            gsc_m = A.get(KC, F32)
            hT = A.get(KC * T, BF16, shape=[KC, T])
            comb = A.get(8 * 32, F32, shape=[8, 32])
            base_mark = A.mark()
            hmT = A.get(H * T, BF16, shape=[H, T])
            oaT = A.get(H * T, BF16, shape=[H, T])
            r1_off = A.mark()
            state = A.get(H * 256, F32, shape=[H, 256])
            state_b = A.get(H * 256, BF16, shape=[H, 256])
            P.memset(state, 0.0, eng="pool")
            P.memset(state_b, 0.0, eng="pool")
            ckvnT = A.get(2 * 2 * T, BF16, shape=[2, 2 * T])
            krT = A.get(2 * T, BF16, parts=64)
            kpesq = A.get(2 * T, BF16, parts=64)
            cosT = A.get(2 * T, BF16, parts=64)
            sinT = A.get(2 * T, BF16, parts=64)
            qhalo = A.get(H * 3, F32, shape=[H, 3])
            khalo = A.get(H * 3, F32, shape=[H, 3])
            carry = A.get(4, F32, parts=8)
            P.memset(carry, 0.0, eng="pool")

            cT = A.get(KC, F32)
            P.dma(cT, cT_d)
            cond = A.get(KC, BF16)
            P.act(cond, cT, AF.Silu)
            P.phase = "rope"
            m2 = A.mark()
            posi = A.get(2 * T, I32, parts=64)
            P.dma(posi, pos_d[0:1, :].partition_broadcast(64) if False else pos_d.to_broadcast([64, 2 * T]))
            ang = A.get(2 * T, F32, parts=64)
            P.copy(ang, posi)
            P.ts(ang, ang, invf[0:64, :], None, ALU.mult)
            for (dst, shift, sg) in ((sinT, 0.0, True), (cosT, math.pi / 2, False)):
                m2b = A.mark()
                y = A.get(2 * T, F32, parts=64)
                kf = A.get(2 * T, F32, parts=64)
                ki = A.get(2 * T, I32, parts=64)
                P.ts(y, ang, shift, None, ALU.add)
                P.ts(kf, y, 1.0 / (2 * math.pi), None, ALU.mult)
                P.copy(ki, kf)
                P.copy(kf, ki)
                c1, c2, c3 = 6.28125, 1.9350051879882812e-3, 3.0199160695e-7
                P.stt(y, kf, -c1, y, ALU.mult, ALU.add)
                P.stt(y, kf, -c2, y, ALU.mult, ALU.add)
                P.stt(y, kf, -c3, y, ALU.mult, ALU.add)
                wr = A.get(2 * T, F32, parts=64)
                P.ts(wr, y, math.pi, -2 * math.pi, ALU.is_gt, ALU.mult)
                P.tt(y, y, wr, ALU.add)
                P.ts(wr, y, -math.pi, 2 * math.pi, ALU.is_lt, ALU.mult)
                P.tt(y, y, wr, ALU.add)
                P.ts(y, y, 3.1415925, -3.1415925, ALU.min, ALU.max)
                if sg:
                    sf = A.get(2 * T, F32, parts=64)
                    P.act(sf, y, AF.Sin)
                    P.ts(dst, sf, sgn[0:64, :], None, ALU.mult)
                else:
                    P.act(dst, y, AF.Sin)
                A.release(m2b)
            A.release(m2)
            ck(2, [cosT, sinT])

            P.phase = "adaln"
            m1 = A.mark()
            modps, modps_i = PS.hold()
            wbufs = [t_.rearrange("p a b -> p (a b)").rearrange("p (k n) -> p k n", n=512) for t_ in (hmT, oaT)]
            rowb = [A.get(512, F32, parts=1) for _ in range(1)]
            badab = [A.get(512, F32, parts=1) for _ in range(1)]
            wsel = [0]

            gbuf = {}

            def adaln_load(g, single=False):
                wi_ = 1 if single else (wsel[0] % 2)
                wsel[0] += 1
                gbuf[g] = wi_
                P.dma(wbufs[wi_], kview(w_ada, g * 512, (g + 1) * 512), queue="pool")

            def adaln_group(g, load=True):
                ph = P.phase
                P.phase = "adaln"
                if load:
                    adaln_load(g)
                wt = wbufs[gbuf[g]]
                bb = badab[0]
                P.dma(bb, b_ada[0:1, g * 512:(g + 1) * 512])
                pr = PS.bank(parts=1)
                for kc in range(KC):
                    P.mm(pr, cond[:, kc:kc + 1], wt[:, kc, :], start=(kc == 0), stop=False)
                P.mm(pr, ones_f[0:1, 0:1], bb, start=False, stop=True)
                row = rowb[0]
                P.copy(row, pr, eng="act")
                for j in range(4):
                    c = g * 4 + j
                    P.mm(modps[:, c:c + 1], row[0:1, j * 128:(j + 1) * 128], ones_f[0:1, 0:1])
                if 8 <= g < 12 or 20 <= g < 24:
                    gg = (g - 8) if g < 12 else (g - 20 + 4)
                    P.dma(gt_d[0:1, gg * 512:(gg + 1) * 512], row, dram_w=("gt%d" % gg,))
                P.phase = ph

            bg_pending = list(range(8, 24))

            bg_loaded = []

            def bg_load(n, single=False):
                for _ in range(n):
                    if bg_pending:
                        g_ = bg_pending.pop(0)
                        adaln_load(g_, single)
                        bg_loaded.append(g_)

            def bg_compute():
                while bg_loaded:
                    adaln_group(bg_loaded.pop(0), load=False)

            for g in range(8):
                adaln_group(g)
            P.copy(modT[:, 0:32], modps[:, 0:32])
            P.stt(gsc_a, modT[:, 16:32], 1.0, smallp[:, SP_GMIX:SP_GMIX + 16], ALU.add, ALU.mult)
            sh_a = modT[:, 0:16]
            sh_m = modT[:, 48:64]

            def norm_transpose(src_dram, gsc, sh, dst_hT, xkeep=None):
                mk = A.mark()
                xb = [A.get(D, F32) for _ in range(2)]
                xs_l = [A.get(D, BF16) for _ in range(2)]
                junk_l = [A.get(D, BF16) for _ in range(2)]
                ssq_l = [A.get(1, F32) for _ in range(2)]
                rstd_l = [A.get(1, F32) for _ in range(2)]
                for tb in range(T // 128):
                    xt = xb[tb % 2]
                    xs, junk, ssq, rstd = xs_l[tb % 2], junk_l[tb % 2], ssq_l[tb % 2], rstd_l[tb % 2]
                    import os
                    ksub = int(os.environ.get("KSUB", "0"))
                    P.dma(xt, src_dram[tb * 128:(tb + 1) * 128, :])
                    if ksub == 1:
                        ck(3, [xt])
                    P.act(junk, xt, AF.Square, accum_out=ssq)
                    if ksub == 2:
                        ck(3, [ssq, junk])
                    P.act(rstd, ssq, AF.Sqrt, bias=C_EPS, scale=1.0 / D)
                    P.recip(rstd, rstd)
                    if ksub == 3:
                        ck(3, [ssq, rstd])
                    P.act(xs, xt, AF.Copy, scale=rstd)
                    if ksub == 4:
                        ck(3, [ssq, rstd, xs])
                    for half in range(2):
                        pb = PS.bank(BF16)
                        for j in range(8):
                            kc = half * 8 + j
                            P.tr(pb[:, j * 128:(j + 1) * 128], xs[:, kc * 128:(kc + 1) * 128], ident_b)
                        for j in range(8):
                            kc = half * 8 + j
                            if j % 2 == 0:
                                P.ts(dst_hT[:, kc, tb * 128:(tb + 1) * 128], pb[:, j * 128:(j + 1) * 128],
                                     gsc[:, kc:kc + 1], sh[:, kc:kc + 1], ALU.mult, ALU.add)
                            else:
                                P.act(dst_hT[:, kc, tb * 128:(tb + 1) * 128], pb[:, j * 128:(j + 1) * 128], AF.Identity,
                                      bias=sh[:, kc:kc + 1], scale=gsc[:, kc:kc + 1])
                        if ksub == 5:
                            ck(3, [dst_hT[:, 0, :]])
                A.release(mk)

            wq = [0]

            def load_w(w, c0, n, nk=KC, bufs=None):
                wt = bufs[wq[0] % len(bufs)]
                wq[0] += 1
                v = wt[:, 0:nk, 0:n]
                P.dma(v, w.rearrange("(kc p) n -> p kc n", p=128)[:, :, c0:c0 + n], queue="pool")
                return v

            def fm_proj(wt, m, rhsT, t0, n, nk=KC, m0=0):
                pb = PS.bank()
                o = pb[0:m, 0:n]
                for kc in range(nk):
                    P.mm(o, wt[:, kc, m0:m0 + m], rhsT[:, kc, t0:t0 + n], start=(kc == 0), stop=(kc == nk - 1))
                return o

            def rms_bc(sq_list, nfeat, n, scratch=None):
                pb = PS.bank()
                o = pb[:, 0:n]
                for i, sq in enumerate(sq_list):
                    k = sq.shape[0]
                    P.mm(o, ones_b[0:k, :], sq, start=(i == 0), stop=(i == len(sq_list) - 1))
                r = A.get(n, F32)
                r2 = scratch if scratch is not None else A.get(n, F32)
                P.act(r2, o, AF.Ln, bias=C_EPS, scale=1.0 / nfeat)
                P.act(r, r2, AF.Exp, scale=-0.5)
                return r

            def gate_phase(tok0, is_ctx, tabs):
                mk = A.mark()
                wg = A.get(KC * 16, BF16, shape=[KC, 16])
                P.dma(wg, kview(w_in, C_I, C_I + 16), queue="pool")
                li = A.get(T, F32, parts=8)
                sp = A.get(T, F32, parts=8)
                for tg in range(2):
                    pi = fm_proj(wg, 8, hT, tg * 512, 512, m0=0)
                    P.ts(li[:, tg * 512:(tg + 1) * 512], pi, smallp[0:8, SP_BI:SP_BI + 1], None, ALU.add)
                    pf = fm_proj(wg, 8, hT, tg * 512, 512, m0=8)
                    e = A.get(512, F32, parts=8)
                    P.ts(e, pf, smallp[0:8, SP_BF:SP_BF + 1], None, ALU.add)
                    P.act(e, e, AF.Exp, scale=-1.0)
                    P.act(sp[:, tg * 512:(tg + 1) * 512], e, AF.Ln, bias=C_ONE[0:8, :])
                if is_ctx:
                    P.ts(sp, sp, flag[0:8, :], None, ALU.mult)
                    P.ts(li, li, flag[0:8, :], negbig[0:8, :], ALU.mult, ALU.add)
                Bn = A.get(T, F32, parts=8)
                U = tabs["U"] if "U" in tabs else A.get(T, F32, parts=8)
                u = A.get(T, F32, parts=8)
                P.scan(Bn, sp, sp, carry[:, 0:1], ALU.add, ALU.max)
                P.tt(u, li, Bn, ALU.add)
                P.scan(U, u, u, carry[:, 1:2], ALU.max, ALU.max)
                Ue = tabs["Ue"]
                Up = tabs["Up"]
                P.copy(Ue, U.rearrange("p (c l) -> p c l", l=128)[:, :, 127])
                P.copy(Up[:, 0:1], carry[:, 1:2])
                P.copy(Up[:, 1:8], Ue[:, 0:7])
                gh = A.get(T, F32, parts=8)
                P.tt(gh.rearrange("p (c l) -> p c l", l=128), u.rearrange("p (c l) -> p c l", l=128),
                     Ue.unsqueeze(2).to_broadcast([8, 8, 128]), ALU.subtract)
                P.act(gh, gh, AF.Exp, bias=C_LNK[0:8, :])
                gT = tabs["gT"]
                pb = PS.bank()
                for c in range(8):
                    P.tr(pb[:, c * 8:(c + 1) * 8], gh[:, c * 128:(c + 1) * 128], ident_f[0:8, 0:8])
                P.copy(gT, pb[:, 0:64].rearrange("p (c h) -> p c h", h=8))
                dec = A.get(8, F32, parts=8)
                P.tt(dec, Up, Ue, ALU.subtract)
                P.act(dec, dec, AF.Exp)
                R = A.get(64, F32, parts=8, shape=[8, 8])
                P.tt(R, dec.unsqueeze(1).to_broadcast([8, 8, 8]), dmask[0:8], ALU.mult)
                pb2 = PS.bank()
                P.mm(pb2[:, 0:64], ones_f[0:8, :], R.rearrange("p a b -> p (a b)"))
                P.copy(tabs["dec_bc"], pb2[:, 0:64].rearrange("p (h c) -> p h c", c=8))
                if not is_ctx:
                    P.tt(tabs["a"].rearrange("p (c l) -> p c l", l=128), Up.unsqueeze(2).to_broadcast([8, 8, 128]),
                         U.rearrange("p (c l) -> p c l", l=128), ALU.subtract)
                    P.act(tabs["a"], tabs["a"], AF.Exp)
                    P.tt(tabs["emt"], Bn, U, ALU.subtract)
                    P.act(tabs["emt"], tabs["emt"], AF.Exp)
                    ul = A.get(T, F32, parts=8)
                    P.ts(ul, u, C_LNK[0:8, :], None, ALU.add)
                    pb3 = PS.bank()
                    for c in range(8):
                        P.tr(pb3[:, c * 8:(c + 1) * 8], ul[:, c * 128:(c + 1) * 128], ident_f[0:8, 0:8])
                    P.copy(tabs["uT"], pb3[:, 0:64].rearrange("p (c h) -> p c h", h=8))
                P.copy(carry[:, 0:1], Bn[:, T - 1:T])
                P.copy(carry[:, 1:2], U[:, T - 1:T])
                A.release(mk)

            def alloc_tabs(own):
                tb = {}
                for k in (("U", "a", "emt") if own else ()):
                    tb[k] = A.get(T, F32, parts=8)
                tb["Ue"] = A.get(8, F32, parts=8)
                tb["Up"] = A.get(8, F32, parts=8)
                tb["gT"] = A.get(64, F32, shape=[8, 8])
                tb["dec_bc"] = A.get(64, F32, shape=[8, 8])
                if own:
                    tb["uT"] = A.get(64, F32, shape=[8, 8])
                return tb

            def conv_silu(pre, wcol, out_b):
                mkcs = A.mark()
                acc = A.get(T, F32)
                cw = smallp[:, SP_CONVW + wcol * 4: SP_CONVW + wcol * 4 + 4]
                cb = smallp[:, SP_CONVB + wcol: SP_CONVB + wcol + 1]
                P.ts(acc, pre[:, 0:T], cw[:, 0:1], cb, ALU.mult, ALU.add)
                for j in range(1, 4):
                    P.stt(acc, pre[:, j:j + T], cw[:, j:j + 1], acc, ALU.mult, ALU.add)
                P.act(out_b, acc, AF.Silu)
                A.release(mkcs)

            def mla_kv_proj(tok_off):
                mk = A.mark()
                wb = [A.get(KC * 256, BF16, shape=[KC, 256]) for _ in range(1)]
                wkv = load_w(w_in, C_CKV, 256, bufs=wb)
                wpe = A.get(KC * 128, BF16, shape=[KC, 128])
                P.dma(wpe, kview(w_in, C_KPE, C_KPE + 128), queue="pool")
                for tg in range(2):
                    mk2 = A.mark()
                    t0 = tg * 512
                    ck = A.get(2 * 512, BF16, shape=[2, 512])
                    sq = A.get(2 * 512, BF16, shape=[2, 512])
                    for c in range(2):
                        pb = fm_proj(wkv, 128, hT, t0, 512, m0=c * 128)
                        P.copy(ck[:, c, :], pb, eng="act")
                        P.act(sq[:, c, :], pb, AF.Square)
                    r = rms_bc([sq[:, 0, :], sq[:, 1, :]], 256, 512)
                    for c in range(2):
                        P.stt(ckvnT[:, c, tok_off + t0: tok_off + t0 + 512], ck[:, c, :],
                              smallp[:, SP_GKVA + c: SP_GKVA + c + 1], r, ALU.mult, ALU.mult)
                    p1 = fm_proj(wpe, 64, hT, t0, 512, m0=0)
                    p2 = fm_proj(wpe, 64, hT, t0, 512, m0=64)
                    P.act(kpesq[:, tok_off + t0: tok_off + t0 + 512], p1, AF.Square)
                    t1 = A.get(512, F32, parts=64)
                    t2 = A.get(512, F32, parts=64)
                    P.stt(t1, p1, smallp[0:64, SP_GKR:SP_GKR + 1], cosT[:, tok_off + t0: tok_off + t0 + 512], ALU.mult, ALU.mult)
                    P.stt(t2, p2, smallp[0:64, SP_GKRS:SP_GKRS + 1], sinT[:, tok_off + t0: tok_off + t0 + 512], ALU.mult, ALU.mult)
                    P.tt(krT[:, tok_off + t0: tok_off + t0 + 512], t1, t2, ALU.add)
                    A.release(mk2)
                A.release(mk)

            def alloc_wsets(n=512):
                return [A.get(KC * n, BF16, shape=[KC, n]) for _ in range(2)]

            def load_head_w(hh, own, wsets):
                ws = wsets[hh % 2]
                n = 512 if own else 384
                P.dma(ws[:, :, 0:n], kview(w_in, C_HEAD + hh * 512, C_HEAD + hh * 512 + n), queue="pool")

            def mlstm_head(hh, tabs, own, hmT=None, wsets=None):
                mk = A.mark()
                ws_ = wsets[hh % 2]
                wk, wv, wqh = (ws_[:, :, i * 128:(i + 1) * 128] for i in range(3))
                wo = ws_[:, :, 384:512] if own else None
                kpre = A.get(3 + T + 1, BF16)
                if own:
                    P.copy(kpre[:, 0:3], khalo[:, hh, :])
                else:
                    P.memset(kpre[:, 0:3], 0.0)
                for tg in range(2):
                    pb = fm_proj(wk, 128, hT, tg * 512, 512)
                    P.copy(kpre[:, 3 + tg * 512: 3 + (tg + 1) * 512], pb, eng="act")
                if not own:
                    P.ts(khalo[:, hh, :], kpre[:, T:T + 3], flag, None, ALU.mult)
                    pq = fm_proj(wqh, 128, hT, T - 3, 3)
                    P.ts(qhalo[:, hh, :], pq, flag, None, ALU.mult)
                V = A.get(8 * 128, BF16, shape=[8, 128])
                VT = A.get(T, BF16)
                for tg in range(2):
                    pbv = fm_proj(wv, 128, hT, tg * 512, 512)
                    P.copy(VT[:, tg * 512:(tg + 1) * 512], pbv, eng="act")
                KT = A.get(T, BF16)
                conv_silu(kpre, 8 + hh, KT)
                if own:
                    qpre = A.get(3 + T + 1, BF16)
                    P.copy(qpre[:, 0:3], qhalo[:, hh, :])
                    sigo = A.get(T, BF16)
                    for tg in range(2):
                        pb = fm_proj(wqh, 128, hT, tg * 512, 512)
                        P.copy(qpre[:, 3 + tg * 512: 3 + (tg + 1) * 512], pb, eng="act")
                        pb = fm_proj(wo, 128, hT, tg * 512, 512)
                        P.act(sigo[:, tg * 512:(tg + 1) * 512], pb, AF.Sigmoid)
                    QT = A.get(T, BF16)
                    conv_silu(qpre, hh, QT)
                pbv2 = PS.bank(BF16)
                for c in range(8):
                    P.tr(pbv2[:, c * 128:(c + 1) * 128], VT[:, c * 128:(c + 1) * 128], ident_b)
                P.copy(V.rearrange("p a b -> p (a b)"), pbv2, eng="act")
                Kg = A.get(8 * 128, BF16, shape=[8, 128])
                pbk = PS.bank(BF16)
                for c in range(8):
                    P.tr(pbk[:, c * 128:(c + 1) * 128], KT[:, c * 128:(c + 1) * 128], ident_b)
                for c in range(8):
                    P.ts(Kg[:, c, :], pbk[:, c * 128:(c + 1) * 128], tabs["gT"][:, c, hh:hh + 1], None, ALU.mult)
                if own:
                    QaT = A.get(T, BF16)
                    Wx = A.get(512, F32)
                    Wt = A.get(T, BF16)
                    emt_bc = A.get(T, F32)
                    for tg in range(2):
                        sl = slice(tg * 512, (tg + 1) * 512)
                        pa = PS.bank()
                        P.mm(pa, sel[0:8, hh, :], tabs["a"][:, sl])
                        P.tt(QaT[:, sl], QT[:, sl], pa, ALU.mult)
                        pu = PS.bank()
                        P.mm(pu, sel[0:8, hh, :], tabs["U"][:, sl])
                        P.tt(Wx.rearrange("p (c l) -> p c l", l=128),
                             mlmask.unsqueeze(1).to_broadcast([128, 4, 128]),
                             pu.rearrange("p (c l) -> p c l", l=128), ALU.subtract)
                        for j in range(4):
                            c = tg * 4 + j
                            P.act(Wt[:, c * 128:(c + 1) * 128], Wx[:, j * 128:(j + 1) * 128], AF.Exp,
                                  bias=tabs["uT"][:, c, hh:hh + 1])
                        pe_ = PS.bank()
                        P.mm(pe_, sel[0:8, hh, :], tabs["emt"][:, sl])
                        P.copy(emt_bc[:, sl], pe_, eng="act")
                Sm = [A.get(128, BF16) for _ in range(8)] if own else None
                sb_all = A.get(8 * 256, BF16, shape=[8, 256]) if own else None
                if own:
                    P.copy(sb_all[:, 0, :], state_b[:, hh, :], eng="act")
                for c in range(8):
                    cs = slice(c * 128, (c + 1) * 128)
                    if own:
                        sps = PS.bank()
                        P.mm(sps[:, 0:128], KT[:, cs], QT[:, cs])
                        P.tt(Sm[c], sps[:, 0:128], Wt[:, cs], ALU.mult)
                    ups = PS.bank()
                    P.mm(ups[:, 0:128], Kg[:, c, :], V[:, c, :])
                    P.mm(ups[:, 128:256], Kg[:, c, :], ones_b)
                    P.stt(state[:, hh, :], state[:, hh, :], tabs["dec_bc"][:, hh, c:c + 1], ups[:, 0:256], ALU.mult, ALU.add)
                    if own and c < 7:
                        P.copy(sb_all[:, c + 1, :], state[:, hh, :], eng="act")
                    elif not own and c == 7:
                        P.copy(state_b[:, hh, :], state[:, hh, :], eng="act")
                if own:
                    held = []
                    for tg in range(2):
                        hps, hps_i = PS.hold()
                        dps, dps_i = PS.hold()
                        held.append((hps, hps_i, dps, dps_i))
                        for j in range(4):
                            c = tg * 4 + j
                            cs = slice(c * 128, (c + 1) * 128)
                            js = slice(j * 128, (j + 1) * 128)
                            P.mm(hps[:, js], V[:, c, :], Sm[c], start=True, stop=False)
                            P.mm(hps[:, js], sb_all[:, c, 0:128], QaT[:, cs], start=False, stop=True)
                            P.mm(dps[:, js], ones_b, Sm[c], start=True, stop=False)
                            P.mm(dps[:, js], sb_all[:, c, 128:256], QaT[:, cs], start=False, stop=True)
                    for tg in range(2):
                        sl = slice(tg * 512, (tg + 1) * 512)
                        hps, hps_i, dps, dps_i = held[tg]
                        mk3 = A.mark()
                        dm = A.get(512, F32)
                        P.act(dm, dps, AF.Abs)
                        P.tt(dm, dm, emt_bc[:, sl], ALU.max)
                        P.act(dm, dm, AF.Ln)
                        P.act(dm, dm, AF.Exp, scale=-1.0)
                        hn = A.get(512, F32)
                        P.tt(hn, hps, dm, ALU.mult)
                        sq = A.get(512, BF16)
                        P.act(sq, hn, AF.Square)
                        r = rms_bc([sq], 128, 512, scratch=dm)
                        P.stt(hn, hn, smallp[:, SP_GML + hh: SP_GML + hh + 1], r, ALU.mult, ALU.mult)
                        P.tt(hmT[:, hh, sl], hn, sigo[:, sl], ALU.mult)
                        A.release(mk3)
                        PS.free(hps_i)
                        PS.free(dps_i)
                A.release(mk)

            P.phase = "ctx"
            norm_transpose(x_ctx, gsc_a, sh_a, hT)
            ck(3, [hT[:, 0, :], hT[:, 15, :]])
            mkc = A.mark()
            tabs_c = alloc_tabs(False)
            gate_phase(0, True, tabs_c)
            wsets = alloc_wsets(384)
            load_head_w(0, False, wsets)
            mla_kv_proj(0)
            ck(4, [tabs_c["gT"].rearrange("p a b -> p (a b)"), tabs_c["dec_bc"].rearrange("p a b -> p (a b)"),
                   ckvnT[:, 0, 0:T], ckvnT[:, 1, 0:T], krT[:, 0:T], kpesq[:, 0:T]])
            for hh in range(H):
                bg_load(1)
                if hh + 1 < H:
                    load_head_w(hh + 1, False, wsets)
                mlstm_head(hh, tabs_c, False, wsets=wsets)
                bg_compute()
            ck(5, [state[:, 0, :], state[:, 7, :], khalo.rearrange("p a b -> p (a b)"), qhalo.rearrange("p a b -> p (a b)")])
            A.release(mkc)

            P.phase = "own_mlstm"
            norm_transpose(x_own, gsc_a, sh_a, hT)
            mko = A.mark()
            tabs_o = alloc_tabs(True)
            wsets = alloc_wsets()
            load_head_w(0, True, wsets)
            gate_phase(T, False, tabs_o)
            for hh in range(H):
                bg_load(1, single=True)
                if hh + 1 < H:
                    load_head_w(hh + 1, True, wsets)
                mlstm_head(hh, tabs_o, True, hmT=hmT, wsets=wsets)
                bg_compute()
            assert not bg_pending and not bg_loaded
            P.copy(modT[:, 32:96], modps[:, 32:96])
            PS.free(modps_i)
            P.stt(gsc_m, modT[:, 64:80], 1.0, smallp[:, SP_GFFN:SP_GFFN + 16], ALU.add, ALU.mult)
            ck(6, [hmT[:, 0, :], hmT[:, 7, :], state[:, 0, :]])
            A.release(mko)

            P.phase = "mla"
            mka = A.mark()
            mla_kv_proj(T)
            cqnT = A.get(4 * T, BF16, shape=[4, T])
            mkq = A.mark()
            wcq = A.get(KC * 512, BF16, shape=[KC, 512])
            P.dma(wcq, kview(w_in, C_CQ, C_CQ + 512), queue="pool")
            for tg in range(2):
                mk2 = A.mark()
                t0 = tg * 512
                cq = A.get(4 * 512, BF16, shape=[4, 512])
                sq = A.get(4 * 512, BF16, shape=[4, 512])
                for c in range(4):
                    pb = fm_proj(wcq, 128, hT, t0, 512, m0=c * 128)
                    P.copy(cq[:, c, :], pb, eng="act")
                    P.act(sq[:, c, :], pb, AF.Square)
                r = rms_bc([sq[:, c, :] for c in range(4)], 512, 512)
                for c in range(4):
                    P.stt(cqnT[:, c, t0:t0 + 512], cq[:, c, :], smallp[:, SP_GQA + c: SP_GQA + c + 1], r, ALU.mult, ALU.mult)
                A.release(mk2)
            A.release(mkq)
            wuq = A.get(4 * 1536, BF16, shape=[4, 1536])
            P.dma(wuq, w_uq.rearrange("(kc p) n -> p kc n", p=128), queue="pool")
            wuqs = A.get(4 * 512, BF16, shape=[4, 8, 64])
            P.dma(wuqs.rearrange("p k h c -> p k (h c)"), w_uqs.rearrange("(kc p) n -> p kc n", p=128), queue="pool")
            wukv = A.get(2 * 2048, BF16, shape=[2, 2048])
            P.dma(wukv, w_ukv.rearrange("(kc p) n -> p kc n", p=128), queue="pool")
            hbufs = [dict(qnT=A.get(T, BF16), qrT=A.get(T, BF16, parts=64), knT=A.get(2 * T, BF16),
                          Vh=A.get(16 * 128, BF16, shape=[16, 128]), rk=A.get(16, F32)) for _ in range(2)]
            pT = [A.get(512, BF16) for _ in range(5)]
            rd = A.get(512, F32)
            rd2 = A.get(512, F32)
            pi_ = [0]

            def attn_prologue(hh):
                hb = hbufs[hh % 2]
                qnT, qrT, knT, Vh, rk = hb["qnT"], hb["qrT"], hb["knT"], hb["Vh"], hb["rk"]
                for tg in range(2):
                    mk2 = A.mark()
                    t0 = tg * 512
                    sl = slice(t0, t0 + 512)
                    pn = fm_proj(wuq, 128, cqnT, t0, 512, nk=4, m0=hh * 192)
                    pr = fm_proj(wuq, 64, cqnT, t0, 512, nk=4, m0=hh * 192 + 128)
                    pbs = PS.bank()
                    prs = pbs[0:64, :]
                    for kc in range(4):
                        P.mm(prs, wuqs[:, kc, hh, :], cqnT[:, kc, sl], start=(kc == 0), stop=(kc == 3))
                    sqn = A.get(512, BF16)
                    sqr = A.get(512, BF16, parts=64)
                    P.act(sqn, pn, AF.Square)
                    P.act(sqr, pr, AF.Square)
                    r = rms_bc([sqn, sqr], 192, 512)
                    P.stt(qnT[:, sl], pn, smallp[:, SP_GQN:SP_GQN + 1], r, ALU.mult, ALU.mult)
                    t1 = A.get(512, F32, parts=64)
                    t2 = A.get(512, F32, parts=64)
                    P.stt(t1, pr, smallp[0:64, SP_GQR:SP_GQR + 1], cosT[:, T + t0: T + t0 + 512], ALU.mult, ALU.mult)
                    P.stt(t2, prs, smallp[0:64, SP_GQRS:SP_GQRS + 1], sinT[:, T + t0: T + t0 + 512], ALU.mult, ALU.mult)
                    P.tt(t1, t1, t2, ALU.add)
                    P.tt(qrT[:, sl], t1, r[0:64, :], ALU.mult)
                    A.release(mk2)
                ssqk, ssqk_i = PS.hold()
                for kg in range(4):
                    mk2 = A.mark()
                    k0 = kg * 512
                    pk = fm_proj(wukv, 128, ckvnT, k0, 512, nk=2, m0=hh * 256)
                    P.act(knT[:, k0:k0 + 512], pk, AF.Copy, scale=smallp[:, SP_GKN:SP_GKN + 1])
                    ksq = A.get(512, BF16)
                    P.act(ksq, pk, AF.Square)
                    for j in range(4):
                        kb = kg * 4 + j
                        P.mm(ssqk[:, kb:kb + 1], ksq[:, j * 128:(j + 1) * 128], ones_b[:, 0:1], start=True, stop=False)
                        P.mm(ssqk[:, kb:kb + 1], kpesq[:, kb * 128:(kb + 1) * 128], ones_b[0:64, 0:1], start=False, stop=True)
                    pv = PS.bank()
                    for j in range(4):
                        kb = kg * 4 + j
                        for kc in range(2):
                            P.mm(pv[:, j * 128:(j + 1) * 128], ckvnT[:, kc, kb * 128:(kb + 1) * 128],
                                 wukv[:, kc, hh * 256 + 128: hh * 256 + 256], start=(kc == 0), stop=(kc == 1))
                    if kg < 2:
                        P.act(Vh[:, kg * 4:(kg + 1) * 4, :], pv.rearrange("p (a b) -> p a b", b=128), AF.Copy, scale=flag)
                    else:
                        P.copy(Vh[:, kg * 4:(kg + 1) * 4, :], pv.rearrange("p (a b) -> p a b", b=128), eng="act")
                    A.release(mk2)
                P.act(rk, ssqk[:, 0:16], AF.Sqrt, bias=C_EPS, scale=1.0 / 192)
                P.recip(rk, rk)
                P.ts(rk, rk, 192 ** -0.5, None, ALU.mult)
                PS.free(ssqk_i)

            def attn_main(hh):
                hb = hbufs[hh % 2]
                qnT, qrT, knT, Vh, rk = hb["qnT"], hb["qrT"], hb["knT"], hb["Vh"], hb["rk"]
                pi = pi_[0]
                for tg in range(2):
                    sl = slice(tg * 512, (tg + 1) * 512)
                    kbs = list(range(8)) + [8 + j for j in range(4 * (tg + 1))]
                    nps, nps_i = PS.hold()
                    dps, dps_i = PS.hold()
                    pend = []
                    for i in range(len(kbs) + 3):
                        if i < len(kbs):
                            kb = kbs[i]
                            ks = slice(kb * 128, (kb + 1) * 128)
                            sps = PS.bank()
                            P.mm(sps, knT[:, ks], qnT[:, sl], start=True, stop=False)
                            P.mm(sps, krT[:, ks], qrT[:, sl], start=False, stop=True)
                            pt = pT[pi % 5]
                            pi += 1
                            P.act(pt, sps, AF.Exp, scale=rk[:, kb:kb + 1])
                            jd = kb - 8 - 4 * tg
                            if jd >= 0:
                                P.tt(pt, pt, maskA[:, jd, :], ALU.mult)
                            pend.append((i, kb, pt))
                        if i >= 3:
                            i_, kb_, pt_ = pend.pop(0)
                            P.mm(nps, Vh[:, kb_, :], pt_, start=(i_ == 0), stop=(i_ == len(kbs) - 1))
                            P.mm(dps, flagones if kb_ < 8 else ones_b, pt_, start=(i_ == 0), stop=(i_ == len(kbs) - 1))
                    P.act(rd2, dps, AF.Ln)
                    P.act(rd, rd2, AF.Exp, scale=-1.0)
                    P.tt(oaT[:, hh, sl], nps, rd, ALU.mult)
                    PS.free(nps_i)
                    PS.free(dps_i)
                pi_[0] = pi

            attn_prologue(0)
            for hh in range(H):
                if hh + 1 < H:
                    attn_prologue(hh + 1)
                attn_main(hh)
            A.release(mka)

            ck(7, [oaT[:, 0, :], oaT[:, 7, :], hmT[:, 0, :], hmT[:, 7, :]])
            P.phase = "proj"
            mixT = A.view_at(r1_off, KC * T, BF16, shape=[KC, T])
            wo_g = [A.get(KC * 512, BF16, shape=[KC, 512]) for _ in range(3)]
            mkp = A.mark()
            wgb = [A.get(KC * 256, BF16, shape=[KC, 256]) for _ in range(2)]
            wpb = [A.get(8 * 256, BF16, shape=[8, 256]) for _ in range(2)]
            def load_proj_w(c):
                wg_ = wgb[c % 2]
                wp_ = wpb[c % 2]
                P.dma(wg_, kview(w_in, C_G + c * 256, C_G + (c + 1) * 256), queue="pool")
                P.dma(wp_, kview(w_pab, c * 256, (c + 1) * 256), queue="pool")

            load_proj_w(0)
            for c in range(KC):
                wg_ = wgb[c % 2]
                wp_ = wpb[c % 2]
                if c + 1 < KC:
                    load_proj_w(c + 1)
                if c in (3, 7, 11):
                    g_ = c // 4
                    P.dma(wo_g[g_], kview(w_out, g_ * 512, (g_ + 1) * 512), queue="pool")
                for tg in range(2):
                    mk2 = A.mark()
                    t0 = tg * 512
                    pga = fm_proj(wg_, 128, hT, t0, 512, m0=0)
                    sga = A.get(512, F32)
                    P.act(sga, pga, AF.Sigmoid)
                    ppa = fm_proj(wp_, 128, oaT, t0, 512, nk=8, m0=0)
                    ma = A.get(512, F32)
                    P.tt(ma, ppa, sga, ALU.mult)
                    pgb = fm_proj(wg_, 128, hT, t0, 512, m0=128)
                    sgb = A.get(512, F32)
                    P.act(sgb, pgb, AF.Sigmoid)
                    ppb = fm_proj(wp_, 128, hmT, t0, 512, nk=8, m0=128)
                    mb = A.get(512, F32)
                    P.tt(mb, ppb, sgb, ALU.mult)
                    P.tt(mixT[:, c, t0:t0 + 512], ma, mb, ALU.add)
                    A.release(mk2)
            A.release(mkp)

            ck(8, [mixT[:, 0, :], mixT[:, 15, :]])
            P.phase = "wout"
            mkw = A.mark()
            A2 = Alloc(big, r1_off)
            A2.top = base_mark
            gt_a_bc = A2.get(D, F32)
            P.dma(gt_a_bc, gt_d[0:1, 0:D].to_broadcast([128, D]), dram_r=("gt0", "gt1", "gt2", "gt3"))
            wo_g.append(A.get(KC * 512, BF16, shape=[KC, 512]))
            P.dma(wo_g[3], kview(w_out, 3 * 512, 4 * 512), queue="pool")
            wrt = A.get(KC * 36, BF16, shape=[KC, 36])
            P.dma(wrt, w_rt.rearrange("(kc p) n -> p kc n", p=128), queue="pool")
            ring0 = A.view_at(190464, KC * 512, BF16)
            P.dma(ring0.rearrange("p (k n) -> p k n", n=512), w_ge[0].rearrange("(kc p) n -> p kc n", p=128), queue="pool")
            brt = A.get(36, F32)
            P.dma(brt, b_rt.to_broadcast([128, 36]))
            xb = [A2.get(D, F32) for _ in range(2)]
            xs = A2.get(D, BF16)
            junk = A2.get(D, BF16)
            ssq = A.get(1, F32)
            rstd = A.get(1, F32)
            def stage_a(tb):
                ts_ = slice(tb * 128, (tb + 1) * 128)
                xt = xb[tb % 2]
                P.dma(xt, x_own[ts_, :])
                for g in range(4):
                    pb = PS.bank()
                    for kc in range(KC):
                        P.mm(pb, mixT[:, kc, ts_], wo_g[g][:, kc, :], start=(kc == 0), stop=(kc == KC - 1))
                    gs = slice(g * 512, (g + 1) * 512)
                    P.tt(pb, pb, gt_a_bc[:, gs], ALU.mult)
                    P.tt(xt[:, gs], xt[:, gs], pb, ALU.add)
                P.dma(xmid_d[ts_, :], xt, dram_w=("xmid",))

            stage_a(0)
            for tb in range(8):
                ts_ = slice(tb * 128, (tb + 1) * 128)
                xt = xb[tb % 2]
                if tb + 1 < 8:
                    stage_a(tb + 1)
                P.act(junk, xt, AF.Square, accum_out=ssq)
                P.act(rstd, ssq, AF.Sqrt, bias=C_EPS, scale=1.0 / D)
                P.recip(rstd, rstd)
                P.act(xs, xt, AF.Copy, scale=rstd)
                for half in range(2):
                    pb = PS.bank(BF16)
                    for j in range(8):
                        kc = half * 8 + j
                        P.tr(pb[:, j * 128:(j + 1) * 128], xs[:, kc * 128:(kc + 1) * 128], ident_b)
                    for j in range(8):
                        kc = half * 8 + j
                        P.ts(hT[:, kc, ts_], pb[:, j * 128:(j + 1) * 128], gsc_m[:, kc:kc + 1], sh_m[:, kc:kc + 1], ALU.mult, ALU.add)
                mk2 = A.mark()
                pl = PS.bank()
                for kc in range(KC):
                    P.mm(pl[:, 0:36], hT[:, kc, ts_], wrt[:, kc, :], start=(kc == 0), stop=(kc == KC - 1))
                lg = A.get(36, F32)
                P.tt(lg, pl[:, 0:36], brt, ALU.add)
                gl = lg[:, 0:4]
                el = lg[:, 4:36].rearrange("p (j e) -> p j e", e=8)
                gmax = A.get(1, F32)
                P.reduce(gmax, gl, ALU.max)
                ngmax = A.get(1, F32)
                P.ts(ngmax, gmax, -1.0, None, ALU.mult)
                ge = A.get(4, F32)
                gsum = A.get(1, F32)
                P.act(ge, gl, AF.Exp, bias=ngmax, accum_out=gsum)
                gw = A.get(1, F32)
                P.recip(gw, gsum)
                oh = A.get(4, F32)
                P.ts(oh, gl, gmax, None, ALU.is_equal)
                esel = A.get(32, F32, shape=[4, 8])
                P.tt(esel, el, oh.unsqueeze(2).to_broadcast([128, 4, 8]), ALU.mult)
                ein = A.get(8, F32)
                P.reduce(ein, esel.rearrange("p j e -> p e j"), ALU.add)
                l1 = A.get(1, F32)
                P.reduce(l1, ein, ALU.max)
                mk1_ = A.get(8, F32)
                P.ts(mk1_, ein, l1, None, ALU.is_equal)
                ein2 = A.get(8, F32)
                P.stt(ein2, mk1_, -1.0e30, ein, ALU.mult, ALU.add)
                l2 = A.get(1, F32)
                P.reduce(l2, ein2, ALU.max)
                mk2_ = A.get(8, F32)
                P.ts(mk2_, ein2, l2, None, ALU.is_equal)
                dd = A.get(1, F32)
                P.tt(dd, l2, l1, ALU.subtract)
                P.act(dd, dd, AF.Exp)
                rr = A.get(1, F32)
                P.ts(rr, dd, 1.0, None, ALU.add)
                P.recip(rr, rr)
                w1 = A.get(1, F32)
                P.tt(w1, gw, rr, ALU.mult)
                w2 = A.get(1, F32)
                P.tt(w2, w1, dd, ALU.mult)
                cg = A.get(8, F32)
                assert A.top <= 190464
                P.ts(cg, mk1_, w1, None, ALU.mult)
                P.stt(cg, mk2_, w2, cg, ALU.mult, ALU.add)
                P.tt(comb[:, tb, :].rearrange("p (j e) -> p j e", e=8), oh.unsqueeze(2).to_broadcast([128, 4, 8]),
                     cg.unsqueeze(1).to_broadcast([128, 4, 8]), ALU.mult)
                A.release(mk2)
            A.release(mkw)

            ck(9, [hT[:, 0, :], hT[:, 15, :], comb.rearrange("p a b -> p (a b)")])
            P.phase = "moe"
            A.release(base_mark)
            gt_m_bc = A.get(D, F32)
            P.dma(gt_m_bc, gt_d[0:1, D:2 * D].to_broadcast([128, D]), dram_r=("gt4", "gt5", "gt6", "gt7"))
            macc = A.get(8 * D, F32, shape=[8, D])
            P.memset(macc, 0.0, eng="pool")
            ring = [ring0] + [A.get(KC * 512, BF16) for _ in range(2)]
            assert A.top <= 190464
            sg = A.get(4 * T, BF16, shape=[4, T])
            ri = 0
            for e in range(N_EXP):
                wgt = ring[ri % 3].rearrange("p (k n) -> p k n", n=512)
                ri += 1
                if e > 0:
                    P.dma(wgt, w_ge[e].rearrange("(kc p) n -> p kc n", p=128), queue="pool")
                wut = ring[ri % 3].rearrange("p (k n) -> p k n", n=512)
                ri += 1
                P.dma(wut, w_ue[e].rearrange("(kc p) n -> p kc n", p=128), queue="pool")
                wdt = ring[ri % 3].rearrange("p (k n) -> p k n", n=D)
                ri += 1
                P.dma(wdt, w_de[e].rearrange("(kc p) n -> p kc n", p=128), queue="pool")
                for fc in range(4):
                    for tg in range(2):
                        pb = fm_proj(wgt, 128, hT, tg * 512, 512, m0=fc * 128)
                        P.act(sg[:, fc, tg * 512:(tg + 1) * 512], pb, AF.Silu)
                for fc in range(4):
                    for tg in range(2):
                        pb = fm_proj(wut, 128, hT, tg * 512, 512, m0=fc * 128)
                        sl = slice(tg * 512, (tg + 1) * 512)
                        P.tt(sg[:, fc, sl], pb, sg[:, fc, sl], ALU.mult)
                for tb in range(8):
                    ts_ = slice(tb * 128, (tb + 1) * 128)
                    for g in range(4):
                        pb = PS.bank()
                        for fc in range(4):
                            P.mm(pb, sg[:, fc, ts_], wdt[:, fc, g * 512:(g + 1) * 512], start=(fc == 0), stop=(fc == 3))
                        gs = slice(g * 512, (g + 1) * 512)
                        P.stt(macc[:, tb, gs], pb, comb[:, tb, e:e + 1], macc[:, tb, gs], ALU.mult, ALU.add)

            P.phase = "final"
            xo = [ring[i].bitcast(F32)[:, 0:D] for i in range(2)]
            for tb in range(8):
                ts_ = slice(tb * 128, (tb + 1) * 128)
                xt = xo[tb % 2]
                P.dma(xt, xmid_d[ts_, :], dram_r=("xmid",))
                P.tt(macc[:, tb, :], macc[:, tb, :], gt_m_bc, ALU.mult, eng="pool")
                P.tt(xt, xt, macc[:, tb, :], ALU.add)
                P.dma(out_d[ts_, :], xt, final=True)

        try:
            _body()
        except _Stop:
            pass
        P.finalize(stack)
        print("PE est us per phase:", {k: int(v) for k, v in getattr(P, "stats", {}).items()})
        print("SBUF peak bytes/partition:", A.peak, " ops:", len(P.ops), {e: len(P.per[e]) for e in P.ENG})
    return nc


def _host_consts():
    c = np.zeros((128, 2048), np.float32)
    c[:, 0:128] = np.eye(128, dtype=np.float32)
    c[:, 128:256] = 1.0
    s = np.arange(128)[:, None]
    t = np.arange(128)[None, :]
    c[:, 256:384] = np.where(s <= t, 0.0, NEG)
    dm = np.zeros((128, 8, 8), np.float32)
    for h in range(8):
        dm[h, h, :] = 1.0
    c[:, 384:448] = dm.reshape(128, 64)
    sl = np.zeros((128, 8, 128), np.float32)
    for h in range(8):
        sl[h, h, :] = 1.0
    c[:, 448:1472] = sl.reshape(128, 1024)
    inv = (10000.0 ** (-np.arange(0, 64, 2, dtype=np.float32) / 64)).astype(np.float32)
    c[0:32, 1472] = inv
    c[32:64, 1472] = inv
    c[0:32, 1473] = -1.0
    c[32:64, 1473] = 1.0
    c[:, 1474] = EPS
    c[:, 1475] = LN_KSCALE
    c[:, 1476] = 1.0
    c[:, 1477] = 0.0
    c[:, 1478] = math.pi / 2
    mA = np.zeros((128, 4, 512), np.float32)
    k = np.arange(128)[:, None]
    q = np.arange(512)[None, :]
    for j in range(4):
        mA[:, j, :] = (128 * j + k <= q)
    return c, mA.reshape(128, 2048)


def _fm(v, nchunk):
    return np.ascontiguousarray(v.reshape(nchunk, 128).T)


_CACHE = {}


def kernel(**inp):
    f32 = np.float32
    x = np.asarray(inp["x"], f32)
    c = np.asarray(inp["c"], f32)
    pos = np.asarray(inp["positions"], np.int32)
    consts, maskA = _host_consts()
    sp = np.zeros((128, 256), f32)
    sp[:, 0:16] = _fm(np.asarray(inp["norm_mix_g"], f32)[0], 16)
    sp[:, 16:32] = _fm(np.asarray(inp["norm_ffn_g"], f32)[0], 16)
    sp[:, 32:36] = _fm(np.asarray(inp["q_a_norm_g"], f32)[0], 4)
    sp[:, 36:38] = _fm(np.asarray(inp["kv_a_norm_g"], f32)[0], 2)
    cw = np.asarray(inp["conv_w"], f32)[0]
    sp[:, 38:102] = cw.reshape(4, 16, 128).transpose(2, 1, 0).reshape(128, 64)
    sp[:, 102:118] = _fm(np.asarray(inp["conv_b"], f32)[0], 16)
    sp[:, 118:126] = _fm(np.asarray(inp["mlstm_norm_g"], f32)[0], 8)
    gq = np.asarray(inp["q_norm_g"], f32)[0]
    gk = np.asarray(inp["k_norm_g"], f32)[0]
    sp[:, 126] = gq[0:128]
    sp[0:64, 127] = gq[128:192]
    sp[0:64, 128] = np.concatenate([gq[160:192], gq[128:160]])
    sp[:, 129] = gk[0:128]
    sp[0:64, 130] = gk[128:192]
    sp[0:64, 131] = np.concatenate([gk[160:192], gk[128:160]])
    bg = np.asarray(inp["b_mlstm_gates"], f32)[0]
    sp[0:8, 132] = bg[0]
    sp[0:8, 133] = bg[1]
    w_rt = np.ascontiguousarray(np.concatenate([np.asarray(inp["w_group"], f32)[0], np.asarray(inp["w_router"], f32)[0]], axis=1))
    b_rt = np.ascontiguousarray(np.concatenate([np.asarray(inp["b_group"], f32)[0], np.asarray(inp["b_router"], f32)[0]])[None, :])
    import os
    wi = np.asarray(inp["w_in"], f32)[0]
    cols = []
    for h in range(8):
        cols += [wi[:, 1856 + h * 128:1856 + (h + 1) * 128], wi[:, 2880 + h * 128:2880 + (h + 1) * 128],
                 wi[:, 832 + h * 128:832 + (h + 1) * 128], wi[:, 3904 + h * 128:3904 + (h + 1) * 128]]
    for c_ in range(16):
        cols += [wi[:, 4944 + c_ * 128:4944 + (c_ + 1) * 128], wi[:, 6992 + c_ * 128:6992 + (c_ + 1) * 128]]
    cols += [wi[:, 0:512], wi[:, 512:768], wi[:, 768:832], wi[:, 800:832], wi[:, 768:800], wi[:, 4928:4944]]
    w_in2 = np.ascontiguousarray(np.concatenate(cols, axis=1))
    assert w_in2.shape == (D, NCOL2)
    wpa = np.asarray(inp["w_proj_a"], f32)[0]
    wpb_ = np.asarray(inp["w_proj_b"], f32)[0]
    w_pab = np.ascontiguousarray(np.concatenate(
        [np.concatenate([wpa[:, c_ * 128:(c_ + 1) * 128], wpb_[:, c_ * 128:(c_ + 1) * 128]], axis=1) for c_ in range(16)], axis=1))
    wq_ = np.asarray(inp["w_uq"], f32)[0]
    w_uqs = np.ascontiguousarray(np.concatenate(
        [np.concatenate([wq_[:, h * 192 + 160:h * 192 + 192], wq_[:, h * 192 + 128:h * 192 + 160]], axis=1) for h in range(8)], axis=1))
    stage = int(os.environ.get("KSTAGE", "99"))
    ne_decl = N_EXP if stage >= 10 else 1
    shared = {
        "D_consts": consts, "D_maskA": maskA, "D_smallp": sp,
        "D_w_ada": np.asarray(inp["w_ada"], f32)[0], "D_b_ada": np.asarray(inp["b_ada"], f32),
        "D_w_in": w_in2, "D_w_uq": np.asarray(inp["w_uq"], f32)[0],
        "D_w_ukv": np.asarray(inp["w_ukv"], f32)[0], "D_w_pab": w_pab, "D_w_uqs": w_uqs, "D_w_out": np.asarray(inp["w_out"], f32)[0],
        "D_w_rt": w_rt, "D_b_rt": b_rt,
        "D_w_gate_e": np.asarray(inp["w_gate_e"], f32)[0][:ne_decl], "D_w_up_e": np.asarray(inp["w_up_e"], f32)[0][:ne_decl],
        "D_w_down_e": np.asarray(inp["w_down_e"], f32)[0][:ne_decl],
    }
    in_maps = []
    for core in range(8):
        b, hf = core // 2, core % 2
        m = dict(shared)
        m["D_x_own"] = np.ascontiguousarray(x[b, hf * T:(hf + 1) * T])
        m["D_x_ctx"] = np.ascontiguousarray(x[b, 0:T])
        m["D_cT"] = _fm(c[b], 16)
        m["D_pos"] = np.ascontiguousarray(np.concatenate([pos[b, 0:T], pos[b, hf * T:(hf + 1) * T]])[None, :])
        m["D_flag"] = np.full((128, 1), float(hf), f32)
        in_maps.append(m)
    if "nc" not in _CACHE:
        _CACHE["nc"] = build_nc(stage)
    ncores = int(os.environ.get("KCORES", "8"))
    res = run_bass_kernel_spmd(_CACHE["nc"], in_maps[:ncores], core_ids=list(range(ncores)))
    out = np.zeros((NB, S, D), f32)
    for core in range(ncores):
        b, hf = core // 2, core % 2
        out[b, hf * T:(hf + 1) * T] = np.asarray(res.results[core]["D_out"], f32)
    return out
```
